# Optimizing a Trainium2 kernel written in Bass

```python
import math
import jax, jax.numpy as jnp
from jax import lax
import numpy as np

D_MODEL = 1024
BATCH = 2
SEQ = 8192
DEPTH = 2

CHUNK = 64
Q_BLOCK = 128
PLE_DIM = 256
MLA_HEADS = 4
MLA_NOPE = 64
MLA_ROPE = 32
MLA_V = 64
MLA_Q_RANK = 192
MLA_KV_RANK = 128
ROPE_THETA = 10000.0
DIFF_HEADS = 4
DIFF_QK = 64
DIFF_V = 2 * DIFF_QK
SB_HEADS = 4
SB_D = 64
REL_BUCKETS = 32
REL_MAX_DIST = 128
N_EXPERTS = 16
N_GROUPS = 4
EXPERTS_PER_GROUP = N_EXPERTS // N_GROUPS
TOP_K = 2
D_EXPERT = 512
MLA_IN = MLA_Q_RANK + MLA_KV_RANK + MLA_ROPE
DIFF_IN = 2 * DIFF_HEADS * 2 * DIFF_QK + DIFF_HEADS * DIFF_V
SB_IN = 3 * SB_HEADS * SB_D
D_IN = MLA_IN + DIFF_IN + SB_IN
D_MIX = MLA_HEADS * MLA_V + DIFF_HEADS * DIFF_V + SB_HEADS * SB_D
DEEPNORM_ALPHA = (2 * DEPTH) ** 0.25
DEEPNORM_BETA = (8 * DEPTH) ** -0.25
EPS = 1e-5
NEG_INF = -1e30

kernel_name = "hybrid_chunk_causal_mla_diff_stickbreak_moe"


def layer_norm(x, g, b):
    xf = x.astype(jnp.float32)
    mu = jnp.mean(xf, -1, keepdims=True)
    var = jnp.mean(jnp.square(xf - mu), -1, keepdims=True)
    y = (xf - mu) * lax.rsqrt(var + EPS) * g.astype(jnp.float32) + b.astype(jnp.float32)
    return y.astype(x.dtype)


def rms_norm(x, g):
    xf = x.astype(jnp.float32)
    y = xf * lax.rsqrt(jnp.mean(xf * xf, -1, keepdims=True) + EPS) * g.astype(jnp.float32)
    return y.astype(x.dtype)


def rope(x, pos):
    half = x.shape[-1] // 2
    inv = ROPE_THETA ** (-jnp.arange(half, dtype=jnp.float32) / half)
    ang = pos.astype(jnp.float32)[..., None] * inv
    cos, sin = jnp.cos(ang), jnp.sin(ang)
    xf = x.astype(jnp.float32)
    x1, x2 = xf[..., :half], xf[..., half:]
    return jnp.concatenate([x1 * cos - x2 * sin, x1 * sin + x2 * cos], -1).astype(x.dtype)


def t5_bucket(rel):
    nb = REL_BUCKETS // 2
    max_exact = nb // 2
    side = jnp.where(rel > 0, nb, 0)
    n = jnp.abs(rel)
    nf = jnp.maximum(n, 1).astype(jnp.float32)
    large = max_exact + (jnp.log(nf / max_exact) / math.log(REL_MAX_DIST / max_exact)
                         * (nb - max_exact)).astype(jnp.int32)
    large = jnp.minimum(large, nb - 1)
    return side + jnp.where(n < max_exact, n, large)


def to_heads(a, n_heads):
    B, S, _ = a.shape
    return a.reshape(B, S, n_heads, -1).transpose(0, 2, 1, 3)


def merge_heads(a):
    B, H, S, d = a.shape
    return a.transpose(0, 2, 1, 3).reshape(B, S, H * d)


def chunk_mask(n, S):
    q_idx = n * Q_BLOCK + jnp.arange(Q_BLOCK)
    k_idx = jnp.arange(S)
    return (k_idx[None, :] // CHUNK) <= (q_idx[:, None] // CHUNK)


def sweep_query_blocks(block_fn, q_parts):
    B, H, S, _ = q_parts[0].shape
    nb = S // Q_BLOCK

    def to_blocks(a):
        return jnp.moveaxis(a.reshape(B, H, nb, Q_BLOCK, a.shape[-1]), 2, 0)

    out = lax.map(lambda args: block_fn(args[0], *args[1:]),
                  (jnp.arange(nb, dtype=jnp.int32),) + tuple(to_blocks(a) for a in q_parts))
    return jnp.moveaxis(out, 0, 2).reshape(B, H, S, out.shape[-1])


def mla_mixer(u, positions, q_norm, w_uq, kv_norm, w_ukv):
    S = u.shape[1]
    c_q, c_kv, k_r = jnp.split(u, [MLA_Q_RANK, MLA_Q_RANK + MLA_KV_RANK], axis=-1)
    q = to_heads(rms_norm(c_q, q_norm) @ w_uq, MLA_HEADS)
    q_nope = q[..., :MLA_NOPE]
    q_rope = rope(q[..., MLA_NOPE:], positions[:, None, :])
    kv = to_heads(rms_norm(c_kv, kv_norm) @ w_ukv, MLA_HEADS)
    k_nope, v = kv[..., :MLA_NOPE], kv[..., MLA_NOPE:]
    k_rope = rope(k_r, positions)
    scale = (MLA_NOPE + MLA_ROPE) ** -0.5

    def block(n, qn, qr):
        s = (jnp.einsum('bhqd,bhkd->bhqk', qn, k_nope)
             + jnp.einsum('bhqd,bkd->bhqk', qr, k_rope)).astype(jnp.float32) * scale
        a = jax.nn.softmax(jnp.where(chunk_mask(n, S), s, NEG_INF), axis=-1)
        return jnp.einsum('bhqk,bhkd->bhqd', a.astype(v.dtype), v)

    return merge_heads(sweep_query_blocks(block, (q_nope, q_rope)))


def diff_mixer(u, positions, rel_bias, lq1, lk1, lq2, lk2, subln, lam_init):
    B, S, _ = u.shape
    qk_w = DIFF_HEADS * 2 * DIFF_QK
    q, k, v = jnp.split(u, [qk_w, 2 * qk_w], axis=-1)
    q = q.reshape(B, S, DIFF_HEADS, 2, DIFF_QK).transpose(3, 0, 2, 1, 4)
    k = k.reshape(B, S, DIFF_HEADS, 2, DIFF_QK).transpose(3, 0, 2, 1, 4)
    v = to_heads(v, DIFF_HEADS)
    f = lambda a: a.astype(jnp.float32)
    lam = jnp.exp(jnp.sum(f(lq1) * f(lk1))) - jnp.exp(jnp.sum(f(lq2) * f(lk2))) + lam_init
    scale = DIFF_QK ** -0.5

    def block(n, q1, q2):
        mask = chunk_mask(n, S)
        pos_blk = lax.dynamic_slice_in_dim(positions, n * Q_BLOCK, Q_BLOCK, axis=1)
        rel = positions[:, None, :] - pos_blk[:, :, None]
        bias = jnp.moveaxis(rel_bias[t5_bucket(rel)], -1, 1).astype(jnp.float32)

        def attn_map(qq, kk):
            s = jnp.einsum('bhqd,bhkd->bhqk', qq, kk).astype(jnp.float32) * scale + bias
            return jax.nn.softmax(jnp.where(mask, s, NEG_INF), axis=-1)

        a = attn_map(q1, k[0]) - lam * attn_map(q2, k[1])
        return jnp.einsum('bhqk,bhkd->bhqd', a.astype(v.dtype), v)

    o = sweep_query_blocks(block, (q[0], q[1]))
    o = rms_norm(o, subln) * (1.0 - lam_init)
    return merge_heads(o)


def sb_mixer(u):
    S = u.shape[1]
    q, k, v = [to_heads(a, SB_HEADS) for a in jnp.split(u, 3, axis=-1)]
    scale = SB_D ** -0.5

    def block(n, qb):
        q_idx = n * Q_BLOCK + jnp.arange(Q_BLOCK)
        strict = jnp.arange(S)[None, :] < q_idx[:, None]
        z = jnp.einsum('bhqd,bhkd->bhqk', qb, k).astype(jnp.float32) * scale
        log_fail = jnp.where(strict, jax.nn.log_sigmoid(-z), 0.0)
        later = lax.cumsum(log_fail, axis=3, reverse=True) - log_fail
        w = jnp.where(strict, jnp.exp(jax.nn.log_sigmoid(z) + later), 0.0)
        return jnp.einsum('bhqk,bhkd->bhqd', w.astype(v.dtype), v)

    return merge_heads(sweep_query_blocks(block, (q,)))


def grouped_moe(h, router_w, router_b, w_gate, w_up, w_down):
    scores = jax.nn.sigmoid((h @ router_w).astype(jnp.float32))
    sel = scores + router_b.astype(jnp.float32)
    g = sel.reshape(sel.shape[:-1] + (N_GROUPS, EXPERTS_PER_GROUP))
    group_score = jnp.sum(lax.top_k(g, 2)[0], axis=-1)
    best_group = jnp.argmax(group_score, axis=-1)
    in_group = jnp.arange(N_GROUPS) == best_group[..., None]
    masked = jnp.where(in_group[..., None], g, -jnp.inf).reshape(sel.shape)
    _, idx = lax.top_k(masked, TOP_K)
    wts = jnp.take_along_axis(scores, idx, axis=-1)
    wts = wts / jnp.sum(wts, -1, keepdims=True)
    gate = jnp.sum(jax.nn.one_hot(idx, N_EXPERTS, dtype=jnp.float32) * wts[..., None], axis=-2)
    gate = gate.astype(h.dtype)
    out = jnp.zeros_like(h)
    for e in range(N_EXPERTS):
        hid = jax.nn.silu(h @ w_gate[e]) * (h @ w_up[e])
        out = out + gate[..., e:e + 1] * (hid @ w_down[e])
    return out


def setup_inputs(seed: int = 0) -> dict:
    key = jax.random.key(seed)
    ks = jax.random.split(key, 32)
    f32 = jnp.float32

    def nrm(k, shape, scale):
        return jax.random.normal(k, shape, f32) * scale

    x = nrm(ks[0], (BATCH, SEQ, D_MODEL), 1.0)
    p = nrm(ks[1], (DEPTH, BATCH, SEQ, PLE_DIM), 1.0)
    offset = jax.random.randint(ks[2], (BATCH, 1), 0, 64, dtype=jnp.int32) * CHUNK
    positions = (jnp.arange(SEQ, dtype=jnp.int32)[None, :] + offset).astype(jnp.int32)
    return {
        'x': x,
        'p': p,
        'positions': positions,
        'w_in': nrm(ks[3], (DEPTH, D_MODEL, D_IN), D_MODEL ** -0.5),
        'mla_q_norm': 1.0 + nrm(ks[4], (DEPTH, MLA_Q_RANK), 0.05),
        'mla_w_uq': nrm(ks[5], (DEPTH, MLA_Q_RANK, MLA_HEADS * (MLA_NOPE + MLA_ROPE)), MLA_Q_RANK ** -0.5),
        'mla_kv_norm': 1.0 + nrm(ks[6], (DEPTH, MLA_KV_RANK), 0.05),
        'mla_w_ukv': nrm(ks[7], (DEPTH, MLA_KV_RANK, MLA_HEADS * (MLA_NOPE + MLA_V)), MLA_KV_RANK ** -0.5),
        'diff_lambda_q1': nrm(ks[8], (DEPTH, DIFF_QK), 0.1),
        'diff_lambda_k1': nrm(ks[9], (DEPTH, DIFF_QK), 0.1),
        'diff_lambda_q2': nrm(ks[10], (DEPTH, DIFF_QK), 0.1),
        'diff_lambda_k2': nrm(ks[11], (DEPTH, DIFF_QK), 0.1),
        'diff_subln': 1.0 + nrm(ks[12], (DEPTH, DIFF_V), 0.05),
        'rel_bias': nrm(ks[13], (REL_BUCKETS, DIFF_HEADS), 0.5),
        'w_o': nrm(ks[14], (DEPTH, D_MIX, D_MODEL), D_MIX ** -0.5 * DEEPNORM_BETA),
        'ln1_g': 1.0 + nrm(ks[15], (DEPTH, D_MODEL), 0.05),
        'ln1_b': nrm(ks[16], (DEPTH, D_MODEL), 0.02),
        'router_w': nrm(ks[17], (D_MODEL, N_EXPERTS), D_MODEL ** -0.5),
        'router_b': nrm(ks[18], (N_EXPERTS,), 0.01),
        'w_gate': nrm(ks[19], (DEPTH, N_EXPERTS, D_MODEL, D_EXPERT), D_MODEL ** -0.5),
        'w_up': nrm(ks[20], (DEPTH, N_EXPERTS, D_MODEL, D_EXPERT), D_MODEL ** -0.5),
        'w_down': nrm(ks[21], (DEPTH, N_EXPERTS, D_EXPERT, D_MODEL), D_EXPERT ** -0.5 * DEEPNORM_BETA),
        'ple_proj': nrm(ks[22], (DEPTH, PLE_DIM, D_MODEL), PLE_DIM ** -0.5 * DEEPNORM_BETA),
        'ple_gate': nrm(ks[23], (DEPTH, D_MODEL, D_MODEL), D_MODEL ** -0.5),
        'ln2_g': 1.0 + nrm(ks[24], (DEPTH, D_MODEL), 0.05),
        'ln2_b': nrm(ks[25], (DEPTH, D_MODEL), 0.02),
    }


def reference(x, p, positions, w_in, mla_q_norm, mla_w_uq, mla_kv_norm, mla_w_ukv,
              diff_lambda_q1, diff_lambda_k1, diff_lambda_q2, diff_lambda_k2, diff_subln,
              rel_bias, w_o, ln1_g, ln1_b, router_w, router_b, w_gate, w_up, w_down,
              ple_proj, ple_gate, ln2_g, ln2_b):
    h = x
    for i in range(DEPTH):
        u = h @ w_in[i]
        u_mla, u_diff, u_sb = jnp.split(u, [MLA_IN, MLA_IN + DIFF_IN], axis=-1)
        y_mla = mla_mixer(u_mla, positions, mla_q_norm[i], mla_w_uq[i], mla_kv_norm[i], mla_w_ukv[i])
        lam_init = 0.8 - 0.6 * math.exp(-0.3 * i)
        y_diff = diff_mixer(u_diff, positions, rel_bias, diff_lambda_q1[i], diff_lambda_k1[i],
                            diff_lambda_q2[i], diff_lambda_k2[i], diff_subln[i], lam_init)
        y_sb = sb_mixer(u_sb)
        mix = jnp.concatenate([y_mla, y_diff, y_sb], axis=-1) @ w_o[i]
        h = layer_norm(DEEPNORM_ALPHA * h + mix, ln1_g[i], ln1_b[i])
        ffn = grouped_moe(h, router_w, router_b, w_gate[i], w_up[i], w_down[i])
        ple = jax.nn.sigmoid(h @ ple_gate[i]) * (p[i] @ ple_proj[i])
        h = layer_norm(DEEPNORM_ALPHA * h + ffn + ple, ln2_g[i], ln2_b[i])
    return h
```

```python
import math
from contextlib import ExitStack

import numpy as np
import concourse.bass as bass
import concourse.mybir as mybir
from concourse.bass_utils import run_bass_kernel_spmd

F32 = mybir.dt.float32
BF16 = mybir.dt.bfloat16
I32 = mybir.dt.int32
ALU = mybir.AluOpType
AF = mybir.ActivationFunctionType
AX = mybir.AxisListType

D_MODEL = 1024
BATCH = 2
SEQ = 8192
DEPTH = 2
N_CORES = 8
EPS = 1e-5
ALPHA = (2 * DEPTH) ** 0.25
TWO_PI = float(2 * np.pi)
NEG_BIG = -30000.0


class Buf:
    __slots__ = ("name", "w", "r", "dsem", "dcnt")

    def __init__(self, name):
        self.name = name
        self.w = None
        self.r = []
        self.dsem = None
        self.dcnt = 0


class T:
    def __init__(self, t, name):
        self.t = t
        self.b = Buf(name)

    def __getitem__(self, k):
        return self.t[k]


class Prog:
    ENGS = ("pe", "act", "dve", "pool", "sp")

    def __init__(self, nc, same_eng_sync=True):
        self.nc = nc
        self.ops = {e: [] for e in self.ENGS}
        self.cnt = {e: 0 for e in self.ENGS}
        self.sem = {e: nc.alloc_semaphore(name="sem_" + e) for e in self.ENGS}
        self.seen = {e: {} for e in self.ENGS}
        self.same_eng_sync = same_eng_sync
        self.nsem = 0
        self.last = {}
        self.dbufs = []
        self.last_swdge = None
        self.nops = 0
        self.trace = False
        import os
        self.stop = int(os.environ["K_STOP"]) if "K_STOP" in os.environ else None

    def _waits(self, eng, reads, writes, extra=()):
        need = {}

        def add(tok, raw):
            if tok is None:
                return
            s, v = tok
            if s is self.sem[eng]:
                if eng == "pe" or not self.same_eng_sync:
                    return
            if need.get(s, 0) < v:
                need[s] = v

        for b in reads:
            add(b.w, True)
        for b in writes:
            add(b.w, True)
            for t in b.r:
                add(t, False)
        for t in extra:
            add(t, True)
        out = []
        seen = self.seen[eng]
        for s, v in need.items():
            if seen.get(s, 0) < v:
                seen[s] = v
                out.append((s, v))
        return out

    def op(self, eng, fn, reads=(), writes=(), inc=True, extra=()):
        self.nops += 1
        if self.stop is not None and self.nops > self.stop:
            return None
        if self.trace:
            print("OP", self.nops, eng, getattr(fn, "desc", "?"), [getattr(x, "name", None) or x.b.name for x in writes], inc)
        reads = [x.b if isinstance(x, T) else x for x in reads]
        writes = [x.b if isinstance(x, T) else x for x in writes]
        waits = self._waits(eng, reads, writes, extra)
        tok = (self.sem[eng], self.cnt[eng] + 1)
        if inc:
            self.cnt[eng] += 1
        self.ops[eng].append((waits, fn, (self.sem[eng], 1) if inc else None))
        for b in reads:
            b.r.append(tok)
            if len(b.r) > 64:
                b.r = b.r[-64:] if False else _compact(b.r)
        for b in writes:
            b.w = tok
            b.r = []
        self.last[eng] = tok
        return tok

    def dma(self, q, out_ap, in_ap, sbuf, is_load, reads=(), writes=(), extra=(), **kw):
        self.nops += 1
        if self.stop is not None and self.nops > self.stop:
            return None
        sbuf = sbuf.b if isinstance(sbuf, T) else sbuf
        if sbuf.dsem is None:
            sbuf.dsem = self.nc.alloc_semaphore(name="dsem_%d" % self.nsem)
            self.nsem += 1
            self.dbufs.append(sbuf)
        rd = [x.b if isinstance(x, T) else x for x in reads]
        wr = [x.b if isinstance(x, T) else x for x in writes]
        if is_load:
            wr.append(sbuf)
        else:
            rd.append(sbuf)
        if q == "pool" and self.last_swdge is not None:
            extra = list(extra) + [self.last_swdge]
        waits = self._waits(q, rd, wr, extra)
        sbuf.dcnt += 1
        tok = (sbuf.dsem, 16 * sbuf.dcnt)
        self.ops[q].append((waits, I("dma_start", out_ap, in_ap, **kw), (sbuf.dsem, 16)))
        if q == "pool":
            self.last_swdge = tok
        for b in rd:
            b.r.append(tok)
            if len(b.r) > 64:
                b.r = _compact(b.r)
        for b in wr:
            b.w = tok
            b.r = []
        return tok

    def wait_all(self, eng, toks):
        waits = self._waits(eng, (), (), extra=toks)
        if waits:
            self.ops[eng].append((waits, None, None))

    def all_last(self):
        return [t for t in self.last.values()]

    def finish(self, eng="sp"):
        toks = self.all_last() + [(b.dsem, 16 * b.dcnt) for b in self.dbufs]
        self.wait_all(eng, toks)

    def emit(self):
        nc = self.nc
        with nc.Block() as block:
            def run(engname):
                def f(e):
                    for waits, fn, inc in self.ops[engname]:
                        for s, v in waits:
                            e.wait_ge(s, v)
                        if fn is None:
                            continue
                        ins = fn(e)
                        if inc is not None:
                            ins.then_inc(inc[0], inc[1])
                return f
            block.tensor(run("pe"))
            block.scalar(run("act"))
            block.vector(run("dve"))
            block.gpsimd(run("pool"))
            block.sync(run("sp"))


def _compact(toks):
    best = {}
    for s, v in toks:
        k = id(s)
        if k not in best or best[k][1] < v:
            best[k] = (s, v)
    return list(best.values())


def I(name, *a, **k):
    f = lambda e: getattr(e, name)(*a, **k)
    f.desc = name
    return f


class Ring:
    def __init__(self, items):
        self.items = items
        self.i = 0

    def next(self):
        x = self.items[self.i % len(self.items)]
        self.i += 1
        return x


def _t5_bucket_np(rel):
    nb = 16
    max_exact = 8
    side = np.where(rel > 0, nb, 0)
    n = np.abs(rel)
    nf = np.maximum(n, 1).astype(np.float32)
    large = max_exact + (np.log(nf / np.float32(max_exact)) / np.float32(math.log(128 / max_exact))
                         * np.float32(nb - max_exact)).astype(np.int32)
    large = np.minimum(large, nb - 1)
    return side + np.where(n < max_exact, n, large)


def _attn_consts():
    kp = np.arange(128)[:, None]
    ql = np.arange(512)[None, :]
    cm = np.zeros((128, 17, 512), np.float32)
    for r in range(4):
        kl = 128 * r + kp
        vis = (kl // 64) <= (ql // 64)
        cm[:, r, :] = vis
        cm[:, 4 + r, :] = kl < ql
        cm[:, 8 + r, :] = np.where(vis, 0.0, NEG_BIG)
    buckets = []
    for i, r in enumerate(range(-1, 4)):
        rel = 128 * r + kp - ql
        bi = _t5_bucket_np(rel)
        cm[:, 12 + i, :] = bi
        if r >= 0:
            vis = ((128 * r + kp) // 64) <= (ql // 64)
            present = sorted(set(bi[vis].tolist()))
        else:
            present = sorted(set(bi.reshape(-1).tolist()))
        buckets.append(present)
    mats = np.zeros((128, 3, 128), np.float32)
    mats[:, 0, :] = 1.0
    j = np.arange(128)[:, None]
    k = np.arange(128)[None, :]
    mats[:, 1, :] = np.where(j >= k, -1.0, 0.0)
    mats[:, 2, :] = -1.0
    ropec = np.zeros((96, 2), np.float32)
    inv = (10000.0 ** (-np.arange(16, dtype=np.float32) / 16)).astype(np.float32)
    ropec[64:96, 0] = np.concatenate([inv, inv])
    ropec[64:80, 1] = -1.0
    ropec[80:96, 1] = 1.0
    return cm, buckets, mats, ropec


_CM, _BUCKETS, _MATS, _ROPEC = _attn_consts()
_BIDX = _CM[:, 12:17, :].astype(np.int64)

W_CQ0, W_CQ1, W_CKV, W_KR, W_KRS, W_QD, W_KD, W_VD, W_QS, W_KS, W_VS, W_END = (
    0, 128, 192, 320, 416, 512, 640, 768, 896, 1024, 1152, 1216)


def build_A():
    nc = bass.Bass("TRN2", target_bir_lowering=False)
    S = SEQ
    import os
    NCH = int(os.environ.get('K_NCH', S // 512))
    PHASES = os.environ.get('K_PH', 'mds')
    DBG = os.environ.get('K_DBG', '')

    def din(name, shape, dt=F32):
        return nc.dram_tensor(name, list(shape), dt, kind="ExternalInput").ap()

    hT = din("hT", [1024, S])
    wall = din("wall", [1024, W_END])
    wuq = din("wuq", [192, 192])
    wukv = din("wukv", [128, 128])
    gq = din("gq", [192, 1])
    gkv = din("gkv", [128, 1])
    pos = din("pos", [1, S], I32)
    ropec = din("ropec", [96, 2])
    lamv = din("lamv", [1, 256])
    lamc = din("lamc", [128, 2])
    subln = din("subln", [128, 1])
    rbj = din("rbj", [1, 32])
    cmask = din("cmask", [128, 17, 512])
    biasg = din("biasg", [128, 5, 512])
    cmats = din("cmats", [128, 3, 128])
    yT = nc.dram_tensor("yT", [256, S], BF16, kind="ExternalOutput").ap()

    P = Prog(nc)
    with ExitStack() as es:
        def sb(name, shape, dt=F32):
            return T(es.enter_context(nc.sbuf_tensor(name, list(shape), dt)), name)

        def ps(name):
            return T(es.enter_context(nc.psum_tensor(name, [128, 512], F32)), name)

        rot = Ring([ps("rot%d" % i) for i in range(4)])
        acc = [ps("acc%d" % i) for i in range(4)]

        wallb = sb("wallb", [128, 8, W_END], BF16)
        wuq0f = sb("wuq0f", [128, 192]); wuq1f = sb("wuq1f", [64, 192])
        wuq0 = sb("wuq0", [128, 192], BF16); wuq1 = sb("wuq1", [64, 192], BF16)
        wukvb = sb("wukvb", [128, 128], BF16)
        gq0 = sb("gq0", [128, 1]); gq1 = sb("gq1", [64, 1]); gkvt = sb("gkvt", [128, 1])
        ropect = sb("ropect", [96, 2])
        lamt = sb("lamt", [128, 256]); lamct = sb("lamct", [128, 2]); sublnt = sb("sublnt", [128, 1])
        lams = sb("lams", [128, 8])
        rbt = sb("rbt", [128, 32])
        matsb = sb("matsb", [128, 3, 128], BF16)
        maskb = sb("maskb", [128, 8, 512], BF16)
        biasm = sb("biasm", [128, 5, 512])
        KTm = sb("KTm", [96, S], BF16); KTd = sb("KTd", [128, S], BF16); KTs = sb("KTs", [128, S // 2], BF16)
        Vm = sb("Vm", [128, S // 128, 65], BF16); Vd = sb("Vd", [128, S // 128, 128], BF16)
        onesf = sb("onesf", [128, 128])
        dacc = [sb("dacc%d" % i, [128, 512]) for i in range(2)]
        P.op("pool", I("memset", onesf[:], 1.0), [], [onesf])
        P.op("pool", I("memset", Vm[:, :, 64:65], 1.0), [], [Vm])
        Vs = sb("Vs", [128, S // 128, 64], BF16)
        kvb = [Buf("kv%d" % i) for i in range(NCH)]

        ones_b = matsb[:, 0, :]
        negTp = matsb[:, 1, :]
        negones = matsb[:, 2, :]

        if 'w' not in DBG:
            P.dma("pool", wallb[:], wall.rearrange("(kc p) n -> p kc n", p=128), wallb, True)
        P.dma("sp", wuq0f[:], wuq[0:128, :], wuq0f, True)
        P.dma("sp", wuq1f[:], wuq[128:192, :], wuq1f, True)
        P.dma("pool", wukvb[:], wukv[:, :], wukvb, True)
        P.dma("sp", gq0[:], gq[0:128, :], gq0, True)
        P.dma("sp", gq1[:], gq[128:192, :], gq1, True)
        P.dma("sp", gkvt[:], gkv[:, :], gkvt, True)
        P.dma("sp", ropect[:], ropec[:, :], ropect, True)
        P.dma("sp", lamt[:], lamv[0:1, :].partition_broadcast(128), lamt, True)
        P.dma("sp", lamct[:], lamc[:, :], lamct, True)
        P.dma("sp", sublnt[:], subln[:, :], sublnt, True)
        P.dma("sp", rbt[:], rbj[0:1, :].partition_broadcast(128), rbt, True)
        P.dma("pool", matsb[:], cmats[:, :, :], matsb, True)
        if 'k' not in DBG:
            P.dma("pool", maskb[:], cmask[:, 0:8, :], maskb, True)

        P.op("dve", I("tensor_scalar", wuq0[:], wuq0f[:], gq0[:, 0:1], None, ALU.mult), [wuq0f, gq0], [wuq0])
        P.op("dve", I("tensor_scalar", wuq1[:], wuq1f[:], gq1[:, 0:1], None, ALU.mult), [wuq1f, gq1], [wuq1])

        ltmp = sb("ltmp", [128, 128])
        P.op("dve", I("tensor_tensor", ltmp[:, 0:64], lamt[:, 0:64], lamt[:, 64:128], ALU.mult), [lamt], [ltmp])
        P.op("dve", I("tensor_tensor", ltmp[:, 64:128], lamt[:, 128:192], lamt[:, 192:256], ALU.mult), [lamt, ltmp], [ltmp])
        P.op("dve", I("tensor_reduce", lams[:, 2:4], ltmp[:].rearrange("p (a b) -> p a b", b=64), AX.X, ALU.add), [ltmp], [lams])
        P.op("act", I("activation", lams[:, 4:6], lams[:, 2:4], AF.Exp), [lams], [lams])
        P.op("dve", I("tensor_tensor", lams[:, 6:7], lams[:, 5:6], lams[:, 4:5], ALU.subtract), [lams], [lams])
        P.op("dve", I("tensor_tensor", lams[:, 0:1], lams[:, 6:7], lamct[:, 0:1], ALU.subtract), [lams, lamct], [lams])
        P.op("dve", I("tensor_tensor", lams[:, 1:2], sublnt[:, 0:1], lamct[:, 1:2], ALU.mult), [sublnt, lamct, lams], [lams])

        f1 = sb("f1", [128, 512]); f2 = sb("f2", [128, 512]); f3 = sb("f3", [128, 512]); f4 = sb("f4", [128, 512])
        bit = Ring([f1, f2])
        if 'b' not in DBG:
            P.dma("sp", biasm[:], biasg[:, :, :], biasm, True)
            for i in range(1, 5):
                bi = bit.next()
                P.dma("sp", bi[:], cmask[:, 8 + (i - 1), :], bi, True)
                P.op("dve", I("tensor_tensor", biasm[:, i, :], biasm[:, i, :], bi[:], ALU.add), [bi, biasm], [biasm])

        hTb = Ring([sb("hTb%d" % i, [128, 8, 512], BF16) for i in range(2)])
        posi = sb("posi", [96, 512], I32)
        ra = f1; rb_ = f2; rc = f3
        ri = posi
        Ct = Ring([sb("Ct%d" % i, [96, 512]) for i in range(1)])
        St = Ring([sb("St%d" % i, [96, 512]) for i in range(1)])
        cqb0 = sb("cqb0", [128, 512], BF16); cqb1 = sb("cqb1", [64, 512], BF16)
        sq0 = sb("sq0", [128, 512], BF16); sq1 = sb("sq1", [64, 512], BF16); sqkv = sb("sqkv", [128, 512], BF16)
        ckvf = sb("ckvf", [128, 512]); rstdq = sb("rstdq", [128, 512]); rstdkv = sb("rstdkv", [128, 512])
        ckvn = sb("ckvn", [128, 512], BF16)
        t1 = sb("t1", [96, 512]); t2 = sb("t2", [96, 512])
        QTm = Ring([sb("QTm%d" % i, [96, 512], BF16) for i in range(2)])
        QTd = Ring([sb("QTd%d" % i, [128, 2, 512], BF16) for i in range(2)])
        for t_ in QTd.items:
            P.op("pool", I("memset", t_[:], 0.0), [], [t_])
        QTs = Ring([sb("QTs%d" % i, [128, 2, 512], BF16) for i in range(2)])
        for t_ in QTs.items:
            P.op("pool", I("memset", t_[:], 0.0), [], [t_])
        aTr = Ring([sb("aT%d" % i, [128, 512], BF16) for i in range(4)])
        aTm = Ring([sb("aTm%d" % i, [128, 512], BF16) for i in range(3)])
        tmpr = Ring([sb("tmpb%d" % i, [128, 512]) for i in range(2)])
        er = Ring([sb("e%d" % i, [128, 512]) for i in range(2)])
        spr = Ring([sb("sp%d" % i, [128, 512], BF16) for i in range(4)])
        wr = Ring([sb("w%d" % i, [128, 512], BF16) for i in range(3)])
        srf = sb("srf", [128, 512])
        srb = Ring([sb("srb%d" % i, [128, 512], BF16) for i in range(4)])
        fsq = sb("fsq", [128, 512], BF16)
        yom = Ring([sb("yo%d" % i, [128, 512], BF16) for i in range(3)])
        yod = yom
        yos = yom

        hT_v = hT.rearrange("(kc p) t -> p kc t", p=128)

        def load_h(tc):
            hb = hTb.next()
            P.dma("pool", hb[:], hT_v[:, :, tc * 512:(tc + 1) * 512], hb, True)
            return hb

        def proj(hb, c0, c1, out_ps, M):
            for kc in range(8):
                P.op("pe", I("matmul", out_ps[0:M, :], wallb[:, kc, c0:c1], hb[:, kc, :], start=(kc == 0), stop=(kc == 7)),
                     [wallb, hb], [out_ps], inc=(kc == 7))

        def projv(hb, c0, c1, out_ps, N):
            for sub in range(4):
                for kc in range(8):
                    P.op("pe", I("matmul", out_ps[:, sub * N:(sub + 1) * N], hb[:, kc, sub * 128:(sub + 1) * 128],
                                                                   wallb[:, kc, c0:c1], start=(kc == 0), stop=(kc == 7)),
                         [wallb, hb], [out_ps], inc=(kc == 7 and sub == 3))

        def rstd_from(ssq_ps, out, dim):
            P.op("act", I("activation", out[:], ssq_ps[:], AF.Ln, scale=1.0 / dim, bias=EPS), [ssq_ps], [out])
            P.op("act", I("activation", out[:], out[:], AF.Exp, scale=-0.5), [out], [out])

        def rope_tables(tc):
            C = Ct.next(); Sg = St.next()
            R = slice(64, 96)
            P.dma("sp", posi[R, :], pos[0:1, tc * 512:(tc + 1) * 512].partition_broadcast(32), posi, True)
            P.op("dve", I("tensor_copy", ra[R, :], posi[R, :]), [posi], [ra])
            P.op("dve", I("tensor_scalar", ra[R, :], ra[R, :], ropect[R, 0:1], None, ALU.mult), [ra, ropect], [ra])

            def reduce_sin(dst, shift):
                P.op("dve", I("tensor_scalar", rb_[R, :], ra[R, :], shift, 1.0 / TWO_PI, ALU.add, ALU.mult), [ra], [rb_])
                P.op("dve", I("tensor_copy", ri[R, :], rb_[R, :]), [rb_], [ri])
                P.op("dve", I("tensor_copy", rb_[R, :], ri[R, :]), [ri], [rb_])
                P.op("dve", I("scalar_tensor_tensor", rc[R, :], rb_[R, :], -TWO_PI, ra[R, :], ALU.mult, ALU.add), [rb_, ra], [rc])
                P.op("dve", I("tensor_scalar", rc[R, :], rc[R, :], shift, None, ALU.add), [rc], [rc])
                P.op("dve", I("tensor_scalar", rb_[R, :], rc[R, :], float(np.pi), -TWO_PI, ALU.is_gt, ALU.mult), [rc], [rb_])
                P.op("dve", I("tensor_tensor", rc[R, :], rc[R, :], rb_[R, :], ALU.add), [rc, rb_], [rc])
                P.op("dve", I("tensor_scalar", rb_[R, :], rc[R, :], -float(np.pi), TWO_PI, ALU.is_lt, ALU.mult), [rc], [rb_])
                P.op("dve", I("tensor_tensor", rc[R, :], rc[R, :], rb_[R, :], ALU.add), [rc, rb_], [rc])
                P.op("act", I("activation", dst[R, :], rc[R, :], AF.Sin), [rc], [dst])

            reduce_sin(Sg, 0.0)
            P.op("dve", I("tensor_scalar", Sg[R, :], Sg[R, :], ropect[R, 1:2], None, ALU.mult), [Sg, ropect], [Sg])
            reduce_sin(C, float(np.pi / 2))
            return C, Sg

        def out_dma(src, rows, r0, tc):
            return P.dma("sp", yT[r0:r0 + rows, tc * 512:(tc + 1) * 512], src[0:rows, :], src, False)

        out_toks = []
        hb_next = load_h(0) if 'h' not in DBG else None
        for tc in range(NCH):
            hb = hb_next
            if tc + 1 < NCH:
                hb_next = load_h(tc + 1)
            t0 = tc * 512
            cs = slice(t0, t0 + 512)
            kvbuf = kvb[tc]
            C, Sg = rope_tables(tc)
            R = slice(64, 96)
            qm = QTm.next(); qd = QTd.next(); qs = QTs.next()

            p_cq0 = rot.next(); proj(hb, W_CQ0, W_CQ0 + 128, p_cq0, 128)
            P.op("dve", I("tensor_copy", cqb0[:], p_cq0[:]), [p_cq0], [cqb0])
            P.op("act", I("activation", sq0[:], cqb0[:], AF.Square), [cqb0], [sq0])
            p_cq1 = rot.next(); proj(hb, W_CQ1, W_CQ1 + 64, p_cq1, 64)
            P.op("dve", I("tensor_copy", cqb1[:], p_cq1[0:64, :]), [p_cq1], [cqb1])
            P.op("act", I("activation", sq1[:], cqb1[:], AF.Square), [cqb1], [sq1])
            p_ckv = rot.next(); proj(hb, W_CKV, W_CKV + 128, p_ckv, 128)
            P.op("dve", I("tensor_copy", ckvf[:], p_ckv[:]), [p_ckv], [ckvf])
            P.op("act", I("activation", sqkv[:], ckvf[:], AF.Square), [ckvf], [sqkv])
            p_ssq = rot.next()
            P.op("pe", I("matmul", p_ssq[:], ones_b, sq0[:], start=True, stop=False), [matsb, sq0], [p_ssq], inc=False)
            P.op("pe", I("matmul", p_ssq[:], matsb[0:64, 0, :], sq1[:], start=False, stop=True), [matsb, sq1], [p_ssq])
            rstd_from(p_ssq, rstdq, 192.0)
            p_ssk = rot.next()
            P.op("pe", I("matmul", p_ssk[:], ones_b, sqkv[:], start=True, stop=True), [matsb, sqkv], [p_ssk])
            rstd_from(p_ssk, rstdkv, 128.0)
            P.op("dve", I("scalar_tensor_tensor", ckvn[:], ckvf[:], gkvt[:, 0:1], rstdkv[:], ALU.mult, ALU.mult), [ckvf, gkvt, rstdkv], [ckvn])
            p_kr = rot.next(); proj(hb, W_KR, W_KR + 96, p_kr, 96)
            P.op("dve", I("tensor_tensor", t1[R, :], p_kr[R, :], C[R, :], ALU.mult), [p_kr, C], [t1])
            p_krs = rot.next(); proj(hb, W_KRS, W_KRS + 96, p_krs, 96)
            P.op("dve", I("tensor_tensor", t2[R, :], p_krs[R, :], Sg[R, :], ALU.mult), [p_krs, Sg], [t2])
            P.op("dve", I("tensor_tensor", KTm[R, cs], t1[R, :], t2[R, :], ALU.add), [t1, t2], [kvbuf])
            sc_m = 96.0 ** -0.5
            p_q = rot.next()
            P.op("pe", I("matmul", p_q[0:96, :], wuq0[:, 0:96], cqb0[:], start=True, stop=False), [wuq0, cqb0], [p_q], inc=False)
            P.op("pe", I("matmul", p_q[0:96, :], wuq1[:, 0:96], cqb1[:], start=False, stop=True), [wuq1, cqb1], [p_q])
            P.op("dve", I("scalar_tensor_tensor", qm[0:64, :], p_q[0:64, :], sc_m, rstdq[0:64, :], ALU.mult, ALU.mult), [p_q, rstdq], [qm])
            P.op("dve", I("tensor_tensor", t1[R, :], p_q[R, :], C[R, :], ALU.mult), [p_q, C], [t1])
            p_qs = rot.next()
            P.op("pe", I("matmul", p_qs[0:96, :], wuq0[:, 96:192], cqb0[:], start=True, stop=False), [wuq0, cqb0], [p_qs], inc=False)
            P.op("pe", I("matmul", p_qs[0:96, :], wuq1[:, 96:192], cqb1[:], start=False, stop=True), [wuq1, cqb1], [p_qs])
            P.op("dve", I("tensor_tensor", t2[R, :], p_qs[R, :], Sg[R, :], ALU.mult), [p_qs, Sg], [t2])
            P.op("dve", I("tensor_tensor", t1[R, :], t1[R, :], t2[R, :], ALU.add), [t1, t2], [t1])
            P.op("dve", I("scalar_tensor_tensor", qm[R, :], t1[R, :], sc_m, rstdq[R, :], ALU.mult, ALU.mult), [t1, rstdq], [qm])
            p_kn = rot.next()
            P.op("pe", I("matmul", p_kn[0:64, :], wukvb[:, 0:64], ckvn[:], start=True, stop=True), [wukvb, ckvn], [p_kn])
            P.op("dve", I("tensor_copy", KTm[0:64, cs], p_kn[0:64, :]), [p_kn], [kvbuf])
            p_vm = rot.next()
            for sub in range(4):
                P.op("pe", I("matmul", p_vm[:, sub * 64:(sub + 1) * 64], ckvn[:, sub * 128:(sub + 1) * 128], wukvb[:, 64:128], start=True, stop=True),
                     [wukvb, ckvn], [p_vm], inc=(sub == 3))
            P.op("dve", I("tensor_copy", Vm[:, tc * 4:(tc + 1) * 4, 0:64], p_vm[:, 0:256].rearrange("p (a b) -> p a b", b=64)), [p_vm, Vm], [kvbuf])
            p_qd = rot.next(); proj(hb, W_QD, W_QD + 128, p_qd, 128)
            P.op("dve", I("tensor_scalar", qd[0:64, 0, :], p_qd[0:64, :], 0.125, None, ALU.mult), [p_qd], [qd])
            P.op("dve", I("tensor_scalar", qd[64:128, 1, :], p_qd[64:128, :], 0.125, None, ALU.mult), [p_qd], [qd])
            p_kd = rot.next(); proj(hb, W_KD, W_KD + 128, p_kd, 128)
            P.op("dve", I("tensor_copy", KTd[:, cs], p_kd[:]), [p_kd], [kvbuf])
            p_vd = rot.next(); projv(hb, W_VD, W_VD + 128, p_vd, 128)
            P.op("dve", I("tensor_copy", Vd[:, tc * 4:(tc + 1) * 4, :], p_vd[:].rearrange("p (a b) -> p a b", b=128)), [p_vd], [kvbuf])
            p_qs2 = rot.next(); proj(hb, W_QS, W_QS + 128, p_qs2, 128)
            P.op("dve", I("tensor_scalar", qs[0:64, 0, :], p_qs2[0:64, :], 0.125, None, ALU.mult), [p_qs2], [qs])
            P.op("dve", I("tensor_scalar", qs[64:128, 1, :], p_qs2[64:128, :], 0.125, None, ALU.mult), [p_qs2], [qs])
            p_ks = rot.next(); proj(hb, W_KS, W_KS + 128, p_ks, 128)
            for jj in range(2):
                pc = (2 * tc + jj) * 128
                P.op("dve", I("tensor_copy", KTs[0:64, pc:pc + 128], p_ks[0:64, 256 * jj:256 * jj + 128]), [p_ks], [kvbuf])
                P.op("dve", I("tensor_copy", KTs[64:128, pc:pc + 128], p_ks[64:128, 256 * jj + 128:256 * jj + 256]), [p_ks], [kvbuf])
            p_vs = rot.next(); projv(hb, W_VS, W_VS + 64, p_vs, 64)
            P.op("dve", I("tensor_copy", Vs[:, tc * 4:(tc + 1) * 4, :], p_vs[:, 0:256].rearrange("p (a b) -> p a b", b=64)), [p_vs], [kvbuf])

            nkb = 4 * tc + 4
            LOOK = 2

            def pipeline(items, front, back, per_step=1):
                pend = []
                for n_, it in enumerate(items):
                    pend.append((it, front(it)))
                    if len(pend) > LOOK + per_step - 1:
                        back(*pend.pop(0))
                    if n_ % per_step == per_step - 1:
                        yield
                while pend:
                    back(*pend.pop(0))
                    yield

            def mla_gen(tc=tc, nkb=nkb, qm=qm):
                aO = acc[0]

                def m_front(kb):
                    r = kb - 4 * tc
                    ks = slice(kb * 128, (kb + 1) * 128)
                    sc = rot.next()
                    P.op("pe", I("matmul", sc[:], KTm[0:96, ks], qm[0:96, :], start=True, stop=True), [kvb[kb // 4], qm], [sc])
                    aT = aTm.next()
                    P.op("act", I("activation", aT[:], sc[:], AF.Exp), [sc], [aT])
                    if r >= 0:
                        P.op("pool", I("tensor_tensor", aT[:], aT[:], maskb[:, r, :], ALU.mult), [aT, maskb], [aT])
                    return aT

                def m_back(kb, aT):
                    P.op("pe", I("matmul", aO[0:65, :], Vm[:, kb, :], aT[:], start=(kb == 0), stop=(kb == nkb - 1)),
                         [kvb[kb // 4], aT, Vm], [aO])

                yield from pipeline(range(nkb), m_front, m_back)
                yo = yom.next()
                P.op("dve", I("reciprocal", f1[64:65, :], aO[64:65, :]), [aO], [f1])
                bc = rot.next()
                P.op("pe", I("matmul", bc[0:64, :], onesf[64:65, 0:64], f1[64:65, :], start=True, stop=True), [onesf, f1], [bc])
                P.op("act", I("copy", f2[0:64, :], bc[0:64, :]), [bc], [f2])
                P.op("dve", I("tensor_tensor", yo[0:64, :], aO[0:64, :], f2[0:64, :], ALU.mult), [aO, f2], [yo])
                out_toks.append(out_dma(yo, 64, 0, tc))

            def diff_gen(tc=tc, nkb=nkb, qd=qd):
                def d_front(it):
                    kb, i = it
                    r = kb - 4 * tc
                    ks = slice(kb * 128, (kb + 1) * 128)
                    PR = slice(64 * i, 64 * i + 64)
                    sc = rot.next()
                    P.op("pe", I("matmul", sc[:], KTd[:, ks], qd[:, i, :], start=True, stop=True), [kvb[kb // 4], qd], [sc])
                    aT = aTr.next()
                    if r <= -2:
                        P.op("act", I("activation", aT[:], sc[:], AF.Exp, bias=rbt[:, 15:16]), [sc, rbt], [aT])
                    else:
                        tb = tmpr.next()
                        P.op("dve", I("tensor_tensor", tb[:], sc[:], biasm[:, r + 1, :], ALU.add), [sc, biasm], [tb])
                        P.op("act", I("activation", aT[:], tb[:], AF.Exp), [tb], [aT])
                    return aT

                def d_back(it, aT):
                    kb, i = it
                    P.op("pe", I("matmul", acc[1 + i][:], Vd[:, kb, :], aT[:], start=(kb == 0), stop=(kb == nkb - 1)),
                         [kvb[kb // 4], aT], [acc[1 + i]])
                    eng = "dve" if i == 0 else "pool"
                    if kb == 0:
                        P.op(eng, I("tensor_copy", dacc[i][:], aT[:]), [aT], [dacc[i]])
                    else:
                        P.op(eng, I("tensor_tensor", dacc[i][:], dacc[i][:], aT[:], ALU.add), [aT, dacc[i]], [dacc[i]])

                yield from pipeline([(kb, i) for kb in range(nkb) for i in range(2)], d_front, d_back, per_step=2)
                den = [rot.next(), rot.next()]
                for i in range(2):
                    P.op("pe", I("matmul", den[i][:], onesf[:], dacc[i][:], start=True, stop=True), [onesf, dacc[i]], [den[i]])
                P.op("dve", I("reciprocal", f1[:], den[0][:]), [den[0]], [f1])
                P.op("dve", I("tensor_tensor", f2[:], acc[1][:], f1[:], ALU.mult), [acc[1], f1], [f2])
                P.op("dve", I("reciprocal", f3[:], den[1][:]), [den[1]], [f3])
                P.op("dve", I("tensor_tensor", f4[:], acc[2][:], f3[:], ALU.mult), [acc[2], f3], [f4])
                P.op("dve", I("scalar_tensor_tensor", f2[:], f4[:], lams[:, 0:1], f2[:], ALU.mult, ALU.add), [f4, lams, f2], [f2])
                P.op("act", I("activation", fsq[:], f2[:], AF.Square), [f2], [fsq])
                p_s = rot.next()
                P.op("pe", I("matmul", p_s[:], ones_b, fsq[:], start=True, stop=True), [matsb, fsq], [p_s])
                rstd_from(p_s, f1, 128.0)
                yo = yod.next()
                P.op("dve", I("scalar_tensor_tensor", yo[:], f2[:], lams[:, 1:2], f1[:], ALU.mult, ALU.mult), [f2, lams, f1], [yo])
                out_toks.append(out_dma(yo, 128, 64, tc))

            def sb_gen(tc=tc, nkb=nkb, qs=qs):
                aO = acc[3]
                state = {"srb": None}

                def s_front(kb):
                    r = kb - 4 * tc
                    ks = slice(kb * 128, (kb + 1) * 128)
                    z = rot.next()
                    kp = slice((kb // 2) * 128, (kb // 2) * 128 + 128)
                    P.op("pe", I("matmul", z[:], KTs[:, kp], qs[:, kb % 2, :], start=True, stop=True), [kvb[kb // 4], qs], [z])
                    ee = er.next()
                    P.op("act", I("activation", ee[:], z[:], AF.Exp), [z], [ee])
                    spt = spr.next()
                    P.op("act", I("activation", spt[:], ee[:], AF.Ln, bias=1.0), [ee], [spt])
                    if r >= 0:
                        P.op("pool", I("tensor_tensor", spt[:], spt[:], maskb[:, 4 + r, :], ALU.mult), [spt, maskb], [spt])
                    sb_in = state["srb"]
                    if kb > 0:
                        if kb == nkb - 1:
                            P.op("dve", I("tensor_copy", srf[:], spt[:]), [spt], [srf])
                        else:
                            P.op("dve", I("tensor_tensor", srf[:], srf[:], spt[:], ALU.add), [spt, srf], [srf])
                        nb = srb.next()
                        P.op("dve", I("tensor_copy", nb[:], srf[:]), [srf], [nb])
                        state["srb"] = nb
                    return spt, sb_in

                def s_back(kb, fr):
                    spt, sbc = fr
                    r = kb - 4 * tc
                    first = (kb == nkb - 1)
                    ks = slice(kb * 128, (kb + 1) * 128)
                    lw = rot.next()
                    kp = slice((kb // 2) * 128, (kb // 2) * 128 + 128)
                    P.op("pe", I("matmul", lw[:], KTs[:, kp], qs[:, kb % 2, :], start=True, stop=False), [kvb[kb // 4], qs], [lw], inc=False)
                    if first:
                        P.op("pe", I("matmul", lw[:], negTp, spt[:], start=False, stop=True), [matsb, spt], [lw])
                    else:
                        P.op("pe", I("matmul", lw[:], negTp, spt[:], start=False, stop=False), [matsb, spt], [lw], inc=False)
                        P.op("pe", I("matmul", lw[:], negones, sbc[:], start=False, stop=True), [matsb, sbc], [lw])
                    wt = wr.next()
                    P.op("act", I("activation", wt[:], lw[:], AF.Exp), [lw], [wt])
                    if r >= 0:
                        P.op("pool", I("tensor_tensor", wt[:], wt[:], maskb[:, 4 + r, :], ALU.mult), [wt, maskb], [wt])
                    return wt

                pend = []
                pend2 = []
                for kb in reversed(range(nkb)):
                    pend.append((kb, s_front(kb)))
                    if len(pend) > LOOK:
                        k2, fr = pend.pop(0)
                        pend2.append((k2, s_back(k2, fr)))
                    if len(pend2) > 1:
                        k3, wt = pend2.pop(0)
                        P.op("pe", I("matmul", aO[0:64, :], Vs[:, k3, :], wt[:], start=(k3 == nkb - 1), stop=(k3 == 0)),
                             [kvb[k3 // 4], wt], [aO])
                    yield
                while pend or pend2:
                    if pend:
                        k2, fr = pend.pop(0)
                        pend2.append((k2, s_back(k2, fr)))
                    if len(pend2) > 1 or not pend:
                        k3, wt = pend2.pop(0)
                        P.op("pe", I("matmul", aO[0:64, :], Vs[:, k3, :], wt[:], start=(k3 == nkb - 1), stop=(k3 == 0)),
                             [kvb[k3 // 4], wt], [aO])
                    yield
                yo = yos.next()
                P.op("dve", I("tensor_copy", yo[0:64, :], aO[0:64, :]), [aO], [yo])
                out_toks.append(out_dma(yo, 64, 192, tc))

            gens = []
            if 'm' in PHASES:
                gens.append(mla_gen())
            if 'd' in PHASES:
                gens.append(diff_gen())
            if 's' in PHASES:
                gens.append(sb_gen())
            while gens:
                for g_ in list(gens):
                    try:
                        next(g_)
                    except StopIteration:
                        gens.remove(g_)

        P.wait_all("sp", out_toks)
        P.finish()
        print("A nops", P.nops, {e: P.cnt[e] for e in P.cnt})
        P.emit()
    return nc


def build_B():
    nc = bass.Bass("TRN2", target_bir_lowering=False)
    NT = 2048
    NTT = NT // 128

    def din(name, shape, dt=F32):
        return nc.dram_tensor(name, list(shape), dt, kind="ExternalInput").ap()

    yT = din("yT", [1024, NT], BF16)
    h = din("h", [NT, 1024])
    pT = din("pT", [256, NT])
    w_o = din("w_o", [1024, 1024])
    ple_gate = din("ple_gate", [1024, 1024])
    ple_proj = din("ple_proj", [256, 1024])
    router_w = din("router_w", [1024, 16])
    router_b = din("router_b", [1, 16])
    lnp = din("lnp", [4, 1024])
    w_gate = din("w_gate", [16, 1024, 512])
    w_up = din("w_up", [16, 1024, 512])
    w_down = din("w_down", [16, 512, 1024])
    ident = din("ident", [128, 128])
    hout = nc.dram_tensor("hout", [NT, 1024], F32, kind="ExternalOutput").ap()

    P = Prog(nc)
    with ExitStack() as es:
        def sb(name, shape, dt=F32, stack=es):
            return T(stack.enter_context(nc.sbuf_tensor(name, list(shape), dt)), name)

        def ps(name):
            return T(es.enter_context(nc.psum_tensor(name, [128, 512], F32)), name)

        pss = [ps("ps%d" % i) for i in range(8)]

        R = sb("R", [128, NTT, 1024])
        Rb = [Buf("R%d" % i) for i in range(NTT)]
        h1T = sb("h1T", [128, 8, NT], BF16)
        h1Tb = [Buf("h1T%d" % i) for i in range(NTT)]
        G = sb("G", [128, NTT, 16])
        Gb = [Buf("G%d" % i) for i in range(NTT)]
        identt = sb("identt", [128, 128])
        lng = sb("lng", [128, 4, 1024])
        P.dma("sp", identt[:], ident[:, :], identt, True)
        for i in range(4):
            P.dma("sp", lng[:, i, :], lnp[i:i + 1, :].partition_broadcast(128), lng, True)

        def layer_norm(src, dst, gi, scr, stat):
            P.op("dve", I("tensor_reduce", stat[:, 0:1], src[:], AX.X, ALU.add), [src], [stat])
            P.op("dve", I("tensor_scalar", stat[:, 1:2], stat[:, 0:1], -1.0 / 1024, None, ALU.mult), [stat], [stat])
            P.op("dve", I("tensor_scalar", src[:], src[:], stat[:, 1:2], None, ALU.add), [src, stat], [src])
            P.op("act", I("activation", scr[:], src[:], AF.Square, accum_out=stat[:, 2:3]), [src, stat], [scr, stat])
            P.op("act", I("activation", stat[:, 3:4], stat[:, 2:3], AF.Ln, scale=1.0 / 1024, bias=EPS), [stat], [stat])
            P.op("act", I("activation", stat[:, 3:4], stat[:, 3:4], AF.Exp, scale=-0.5), [stat], [stat])
            P.op("dve", I("scalar_tensor_tensor", dst[:], src[:], stat[:, 3:4], lng[:, gi, :], ALU.mult, ALU.mult), [src, stat, lng], [dst])
            P.op("dve", I("tensor_tensor", dst[:], dst[:], lng[:, gi + 1, :], ALU.add), [dst, lng], [dst])

        with ExitStack() as s1:
            wob = sb("wob", [128, 8, 1024], BF16, s1)
            pgb = sb("pgb", [128, 8, 1024], BF16, s1)
            ppb = sb("ppb", [128, 2, 1024], BF16, s1)
            rwt = sb("rwt", [128, 8, 16], F32, s1)
            rbt = sb("rbt", [128, 16], F32, s1)
            P.dma("pool", wob[:], w_o.rearrange("(kc p) n -> p kc n", p=128), wob, True)
            P.dma("pool", pgb[:], ple_gate.rearrange("(kc p) n -> p kc n", p=128), pgb, True)
            P.dma("pool", ppb[:], ple_proj.rearrange("(kc p) n -> p kc n", p=128), ppb, True)
            P.dma("sp", rwt[:], router_w.rearrange("(kc p) n -> p kc n", p=128), rwt, True)
            P.dma("sp", rbt[:], router_b[0:1, :].partition_broadcast(128), rbt, True)
            yTb = Ring([sb("yTb%d" % i, [128, 8, 128], BF16, s1) for i in range(2)])
            pTb = Ring([sb("pTb%d" % i, [128, 2, 128], BF16, s1) for i in range(2)])
            pTf = Ring([sb("pTf%d" % i, [128, 2, 128], F32, s1) for i in range(2)])
            ht = Ring([sb("ht%d" % i, [128, 1024], F32, s1) for i in range(2)])
            pre = Ring([sb("pre%d" % i, [128, 1024], F32, s1) for i in range(2)])
            h1 = Ring([sb("h1_%d" % i, [128, 1024], F32, s1) for i in range(2)])
            scr = sb("scr", [128, 1024], F32, s1)
            h1Tf = Ring([sb("h1Tf%d" % i, [128, 8, 128], F32, s1) for i in range(2)])
            sg = sb("sg", [128, 1024], F32, s1)
            stat = Ring([sb("stat%d" % i, [128, 8], F32, s1) for i in range(2)])
            rt = [sb("rt%d" % i, [128, 16], F32, s1) for i in range(6)]
            rs = [sb("rs%d" % i, [128, 4], F32, s1) for i in range(6)]
            psr = Ring(pss)

            yT_v = yT.rearrange("(kc p) t -> p kc t", p=128)
            pT_v = pT.rearrange("(kc p) t -> p kc t", p=128)
            def b1_stage1(tt):
                ts_ = slice(tt * 128, (tt + 1) * 128)
                yb = yTb.next(); pb = pTb.next(); hh = ht.next()
                pf = pTf.next()
                P.dma("sp", yb[:], yT_v[:, :, ts_], yb, True)
                P.dma("sp", pf[:], pT_v[:, :, ts_], pf, True)
                P.op("pool", I("tensor_copy", pb[:], pf[:]), [pf], [pb])
                P.dma("sp", hh[:], h[ts_, :], hh, True)
                pr = pre.next(); st = stat.next(); hn = h1.next()
                for half in range(2):
                    pm = psr.next()
                    for kc in range(8):
                        P.op("pe", I("matmul", pm[:], yb[:, kc, :], wob[:, kc, half * 512:(half + 1) * 512],
                                                                                     start=(kc == 0), stop=(kc == 7)), [yb, wob], [pm], inc=(kc == 7))
                    P.op("dve", I("scalar_tensor_tensor",
                        pr[:, half * 512:(half + 1) * 512], hh[:, half * 512:(half + 1) * 512], ALPHA, pm[:], ALU.mult, ALU.add), [hh, pm], [pr])
                layer_norm(pr, hn, 0, scr, st)
                return pb, hn

            def b1_stage2(tt, pb, hn):
                ts_ = slice(tt * 128, (tt + 1) * 128)
                hf = h1Tf.next()
                for half in range(2):
                    pt = psr.next()
                    for k4 in range(4):
                        kc = half * 4 + k4
                        P.op("pe", I("transpose", pt[:, k4 * 128:(k4 + 1) * 128], hn[:, kc * 128:(kc + 1) * 128], identt[:]),
                             [hn, identt], [pt], inc=(k4 == 3))
                    P.op("act", I("copy", hf[:, half * 4:(half + 1) * 4, :], pt[:].rearrange("p (a b) -> p a b", b=128)), [pt], [hf])
                    P.op("dve", I("tensor_copy", h1T[:, half * 4:(half + 1) * 4, ts_], hf[:, half * 4:(half + 1) * 4, :]),
                         [hf], [h1Tb[tt]])
                pl = psr.next()
                for kc in range(8):
                    P.op("pe", I("matmul", pl[:, 0:16], hf[:, kc, :], rwt[:, kc, :], start=(kc == 0), stop=(kc == 7)),
                         [hf, rwt], [pl], inc=(kc == 7))
                sc_, sel, eq, g2, selm, wts = rt
                m1, m2, gs, ing, gmx, wsum = rs
                v3 = lambda a: a[:].rearrange("p (g k) -> p g k", k=4)
                b3 = lambda a: a[:, 0:4].unsqueeze(2).broadcast_to([128, 4, 4])
                P.op("act", I("activation", sc_[:], pl[:, 0:16], AF.Sigmoid), [pl], [sc_])
                P.op("dve", I("tensor_tensor", sel[:], sc_[:], rbt[:], ALU.add), [sc_, rbt], [sel])
                P.op("dve", I("tensor_reduce", m1[:], v3(sel), AX.X, ALU.max), [sel], [m1])
                P.op("dve", I("tensor_tensor", v3(eq), v3(sel), b3(m1), ALU.is_equal), [sel, m1], [eq])
                P.op("dve", I("scalar_tensor_tensor", g2[:], eq[:], -1e9, sel[:], ALU.mult, ALU.add), [eq, sel], [g2])
                P.op("dve", I("tensor_reduce", m2[:], v3(g2), AX.X, ALU.max), [g2], [m2])
                P.op("dve", I("tensor_tensor", gs[:], m1[:], m2[:], ALU.add), [m1, m2], [gs])
                P.op("dve", I("tensor_reduce", gmx[:, 0:1], gs[:], AX.X, ALU.max), [gs], [gmx])
                P.op("dve", I("tensor_scalar", ing[:], gs[:], gmx[:, 0:1], None, ALU.is_equal), [gs, gmx], [ing])
                P.op("dve", I("tensor_tensor", v3(selm), v3(sel), b3(m2), ALU.is_ge), [sel, m2], [selm])
                P.op("dve", I("tensor_tensor", v3(selm), v3(selm), b3(ing), ALU.mult), [selm, ing], [selm])
                P.op("dve", I("tensor_tensor", wts[:], sc_[:], selm[:], ALU.mult), [sc_, selm], [wts])
                P.op("dve", I("tensor_reduce", wsum[:, 0:1], wts[:], AX.X, ALU.add), [wts], [wsum])
                P.op("dve", I("reciprocal", wsum[:, 1:2], wsum[:, 0:1]), [wsum], [wsum])
                P.op("dve", I("tensor_scalar", G[:, tt, :], wts[:], wsum[:, 1:2], None, ALU.mult), [wts, wsum], [Gb[tt]])
                for half in range(2):
                    hs = slice(half * 512, (half + 1) * 512)
                    pg = psr.next()
                    for kc in range(8):
                        P.op("pe", I("matmul", pg[:], h1T[:, kc, ts_], pgb[:, kc, hs], start=(kc == 0), stop=(kc == 7)),
                             [h1Tb[tt], pgb], [pg], inc=(kc == 7))
                    pp = psr.next()
                    for kc in range(2):
                        P.op("pe", I("matmul", pp[:], pb[:, kc, :], ppb[:, kc, hs], start=(kc == 0), stop=(kc == 1)),
                             [pb, ppb], [pp], inc=(kc == 1))
                    P.op("act", I("activation", sg[:, hs], pg[:], AF.Sigmoid), [pg], [sg])
                    P.op("dve", I("tensor_tensor", sg[:, hs], sg[:, hs], pp[:], ALU.mult), [sg, pp], [sg])
                    P.op("dve", I("scalar_tensor_tensor", R[:, tt, hs], hn[:, hs], ALPHA, sg[:, hs], ALU.mult, ALU.add),
                         [hn, sg], [Rb[tt]])

            nxt1 = b1_stage1(0)
            for tt in range(NTT):
                cur1 = nxt1
                if tt + 1 < NTT:
                    nxt1 = b1_stage1(tt + 1)
                b1_stage2(tt, *cur1)
        barrier = P.all_last()

        with ExitStack() as s2:
            wgb = Ring([sb("wgb%d" % i, [128, 8, 512], BF16, s2) for i in range(2)])
            wub = Ring([sb("wub%d" % i, [128, 8, 512], BF16, s2) for i in range(2)])
            wdb = Ring([sb("wdb%d" % i, [128, 4, 1024], BF16, s2) for i in range(2)])
            hid = Ring([sb("hid%d" % i, [128, 4, 512], BF16, s2) for i in range(2)])
            sgr = Ring([sb("sgm%d" % i, [128, 512], F32, s2) for i in range(2)])
            psgu = Ring(pss[0:4])
            psd = Ring(pss[4:8])

            def load_w(e_):
                a, b, c = wgb.next(), wub.next(), wdb.next()
                P.dma("pool", a[:], w_gate[e_].rearrange("(kc p) n -> p kc n", p=128), a, True, extra=barrier)
                P.dma("pool", b[:], w_up[e_].rearrange("(kc p) n -> p kc n", p=128), b, True, extra=barrier)
                P.dma("pool", c[:], w_down[e_].rearrange("(kc p) n -> p kc n", p=128), c, True, extra=barrier)
                return a, b, c

            nxt = load_w(0)
            for e_ in range(16):
                wg_, wu_, wd_ = nxt
                if e_ + 1 < 16:
                    nxt = load_w(e_ + 1)
                for c4 in range(4):
                    cs = slice(c4 * 512, (c4 + 1) * 512)
                    hbufs = [h1Tb[c4 * 4 + i] for i in range(4)]
                    hd = hid.next()
                    for m in range(4):
                        ms = slice(m * 128, (m + 1) * 128)
                        pg = psgu.next(); pu = psgu.next()
                        for kc in range(8):
                            P.op("pe", I("matmul", pg[:], wg_[:, kc, ms], h1T[:, kc, cs], start=(kc == 0), stop=(kc == 7)),
                                 [wg_] + hbufs, [pg], inc=(kc == 7), extra=barrier if (e_ == 0 and c4 == 0 and m == 0 and kc == 0) else ())
                        for kc in range(8):
                            P.op("pe", I("matmul", pu[:], wu_[:, kc, ms], h1T[:, kc, cs], start=(kc == 0), stop=(kc == 7)),
                                 [wu_] + hbufs, [pu], inc=(kc == 7))
                        sg_ = sgr.next()
                        P.op("act", I("activation", sg_[:], pg[:], AF.Silu), [pg], [sg_])
                        P.op("dve", I("tensor_tensor", hd[:, m, :], sg_[:], pu[:], ALU.mult), [sg_, pu], [hd])
                    for t4 in range(4):
                        tt = c4 * 4 + t4
                        for half in range(2):
                            hs = slice(half * 512, (half + 1) * 512)
                            pd = psd.next()
                            for m in range(4):
                                P.op("pe", I("matmul", pd[:], hd[:, m, t4 * 128:(t4 + 1) * 128], wd_[:, m, hs],
                                                                                                        start=(m == 0), stop=(m == 3)), [hd, wd_], [pd], inc=(m == 3))
                            P.op("dve", I("scalar_tensor_tensor", R[:, tt, hs], pd[:], G[:, tt, e_:e_ + 1], R[:, tt, hs], ALU.mult, ALU.add),
                                 [pd, Gb[tt], Rb[tt]], [Rb[tt]])
        barrier2 = P.all_last()

        with ExitStack() as s3:
            ob = Ring([sb("ob%d" % i, [128, 1024], F32, s3) for i in range(2)])
            scr2 = sb("scr2", [128, 1024], F32, s3)
            stat2 = Ring([sb("stat2_%d" % i, [128, 8], F32, s3) for i in range(2)])
            rtmp = Ring([sb("rtmp%d" % i, [128, 1024], F32, s3) for i in range(2)])
            toks = []
            for tt in range(NTT):
                src = rtmp.next(); o = ob.next(); st = stat2.next()
                P.op("pool", I("tensor_copy", src[:], R[:, tt, :]), [Rb[tt]], [src], extra=barrier2 if tt == 0 else ())
                if tt == 0:
                    P.wait_all("dve", barrier2); P.wait_all("act", barrier2)
                layer_norm(src, o, 2, scr2, st)
                toks.append(P.dma("sp", hout[tt * 128:(tt + 1) * 128, :], o[:], o, False))
            P.wait_all("sp", toks)
        P.finish()
        P.emit()
    return nc


_PROGS = {}


def _prog(name):
    if name not in _PROGS:
        _PROGS[name] = build_A() if name == "A" else build_B()
    return _PROGS[name]


def _c(a):
    return np.ascontiguousarray(a, dtype=np.float32)


def _a_inputs(l, c, hT_b, inp):
    b, j = divmod(c, 4)
    w_in = inp["w_in"][l]
    MLA_IN = 352
    D0 = MLA_IN
    S0 = MLA_IN + 1536
    wall = np.zeros((1024, W_END), np.float32)
    wall[:, W_CQ0:W_CQ0 + 192] = w_in[:, 0:192]
    wall[:, W_CKV:W_CKV + 128] = w_in[:, 192:320]
    kr = w_in[:, 320:352]
    wall[:, W_KR + 64:W_KR + 96] = kr
    wall[:, W_KRS + 64:W_KRS + 96] = np.concatenate([kr[:, 16:32], kr[:, 0:16]], axis=1)
    wall[:, W_QD:W_QD + 128] = w_in[:, D0 + j * 128:D0 + (j + 1) * 128]
    wall[:, W_KD:W_KD + 128] = w_in[:, D0 + 512 + j * 128:D0 + 512 + (j + 1) * 128]
    wall[:, W_VD:W_VD + 128] = w_in[:, D0 + 1024 + j * 128:D0 + 1024 + (j + 1) * 128]
    for d_ in (0, 64):
        wall[:, W_QS + d_:W_QS + d_ + 64] = w_in[:, S0 + j * 64:S0 + (j + 1) * 64]
        wall[:, W_KS + d_:W_KS + d_ + 64] = w_in[:, S0 + 256 + j * 64:S0 + 256 + (j + 1) * 64]
    wall[:, W_VS:W_VS + 64] = w_in[:, S0 + 512 + j * 64:S0 + 512 + (j + 1) * 64]
    wq = inp["mla_w_uq"][l][:, j * 96:(j + 1) * 96]
    wuq = np.zeros((192, 192), np.float32)
    wuq[:, 0:96] = wq
    wuq[:, 96 + 64:96 + 80] = wq[:, 80:96]
    wuq[:, 96 + 80:96 + 96] = wq[:, 64:80]
    lam_init = 0.8 - 0.6 * math.exp(-0.3 * l)
    lamc = np.zeros((128, 2), np.float32)
    lamc[:, 0] = lam_init
    lamc[:, 1] = 1.0 - lam_init
    lamv = np.concatenate([inp["diff_lambda_q1"][l], inp["diff_lambda_k1"][l],
                           inp["diff_lambda_q2"][l], inp["diff_lambda_k2"][l]])[None, :]
    return {
        "hT": hT_b,
        "wall": wall,
        "wuq": wuq,
        "wukv": _c(inp["mla_w_ukv"][l][:, j * 128:(j + 1) * 128]),
        "gq": _c(inp["mla_q_norm"][l][:, None]),
        "gkv": _c(inp["mla_kv_norm"][l][:, None]),
        "pos": np.ascontiguousarray(inp["positions"][b][None, :], dtype=np.int32),
        "ropec": _ROPEC,
        "lamv": _c(lamv),
        "lamc": lamc,
        "subln": _c(inp["diff_subln"][l][:, None]),
        "rbj": _c(inp["rel_bias"][:, j][None, :]),
        "cmask": _CM,
        "biasg": _c(inp["rel_bias"][:, j][_BIDX]),
        "cmats": _MATS,
    }


def _b_inputs(l, c, YT, h_flat, inp):
    b, q = divmod(c, 4)
    ts_ = slice(q * 2048, (q + 1) * 2048)
    return {
        "yT": np.ascontiguousarray(YT[b][:, ts_]),
        "h": _c(h_flat[c * 2048:(c + 1) * 2048]),
        "pT": _c(inp["p"][l, b, ts_, :].T),
        "w_o": _c(inp["w_o"][l]),
        "ple_gate": _c(inp["ple_gate"][l]),
        "ple_proj": _c(inp["ple_proj"][l]),
        "router_w": _c(inp["router_w"]),
        "router_b": _c(inp["router_b"][None, :]),
        "lnp": _c(np.stack([inp["ln1_g"][l], inp["ln1_b"][l], inp["ln2_g"][l], inp["ln2_b"][l]])),
        "w_gate": _c(inp["w_gate"][l]),
        "w_up": _c(inp["w_up"][l]),
        "w_down": _c(inp["w_down"][l]),
        "ident": np.eye(128, dtype=np.float32),
    }


def run_A(l, h_flat, inp):
    hT = [_c(h_flat[b * SEQ:(b + 1) * SEQ].T) for b in range(BATCH)]
    in_maps = [_a_inputs(l, c, hT[c // 4], inp) for c in range(N_CORES)]
    res = run_bass_kernel_spmd(_prog("A"), in_maps, core_ids=list(range(N_CORES)))
    YT = [np.zeros((1024, SEQ), res.results[0]["yT"].dtype) for _ in range(BATCH)]
    for c in range(N_CORES):
        b, j = divmod(c, 4)
        y = res.results[c]["yT"]
        YT[b][j * 64:(j + 1) * 64] = y[0:64]
        YT[b][256 + j * 128:256 + (j + 1) * 128] = y[64:192]
        YT[b][768 + j * 64:768 + (j + 1) * 64] = y[192:256]
    return YT


def run_B(l, YT, h_flat, inp):
    in_maps = [_b_inputs(l, c, YT, h_flat, inp) for c in range(N_CORES)]
    res = run_bass_kernel_spmd(_prog("B"), in_maps, core_ids=list(range(N_CORES)))
    return np.concatenate([res.results[c]["hout"] for c in range(N_CORES)], axis=0)


def kernel(**inputs):
    inp = {k: np.asarray(v) for k, v in inputs.items()}
    h_flat = _c(inp["x"].reshape(BATCH * SEQ, D_MODEL))
    for l in range(DEPTH):
        YT = run_A(l, h_flat, inp)
        h_flat = run_B(l, YT, h_flat, inp)
    return h_flat.reshape(BATCH, SEQ, D_MODEL).astype(np.float32)
```

```python
import math
from contextlib import ExitStack

import numpy as np
import concourse.bass as bass
import concourse.mybir as mybir
from concourse.bass_utils import run_bass_kernel_spmd

F32 = mybir.dt.float32
BF16 = mybir.dt.bfloat16
I32 = mybir.dt.int32
ALU = mybir.AluOpType
AF = mybir.ActivationFunctionType
AX = mybir.AxisListType

D_MODEL = 1024
BATCH = 2
SEQ = 8192
DEPTH = 2
N_CORES = 8
EPS = 1e-5
ALPHA = (2 * DEPTH) ** 0.25
TWO_PI = float(2 * np.pi)
NEG_BIG = -30000.0


class Buf:
    __slots__ = ("name", "w", "r", "dsem", "dcnt")

    def __init__(self, name):
        self.name = name
        self.w = None
        self.r = []
        self.dsem = None
        self.dcnt = 0


class T:
    def __init__(self, t, name):
        self.t = t
        self.b = Buf(name)

    def __getitem__(self, k):
        return self.t[k]


class Prog:
    ENGS = ("pe", "act", "dve", "pool", "sp")

    def __init__(self, nc, same_eng_sync=True):
        self.nc = nc
        self.ops = {e: [] for e in self.ENGS}
        self.cnt = {e: 0 for e in self.ENGS}
        self.sem = {e: nc.alloc_semaphore(name="sem_" + e) for e in self.ENGS}
        self.seen = {e: {} for e in self.ENGS}
        self.same_eng_sync = same_eng_sync
        self.nsem = 0
        self.last = {}
        self.dbufs = []
        self.last_swdge = None
        self.nops = 0
        self.trace = False
        import os
        self.stop = int(os.environ["K_STOP"]) if "K_STOP" in os.environ else None

    def _waits(self, eng, reads, writes, extra=()):
        need = {}

        def add(tok, raw):
            if tok is None:
                return
            s, v = tok
            if s is self.sem[eng]:
                if eng == "pe" or not self.same_eng_sync:
                    return
            if need.get(s, 0) < v:
                need[s] = v

        for b in reads:
            add(b.w, True)
        for b in writes:
            add(b.w, True)
            for t in b.r:
                add(t, False)
        for t in extra:
            add(t, True)
        out = []
        seen = self.seen[eng]
        for s, v in need.items():
            if seen.get(s, 0) < v:
                seen[s] = v
                out.append((s, v))
        return out

    def op(self, eng, fn, reads=(), writes=(), inc=True, extra=()):
        self.nops += 1
        if self.stop is not None and self.nops > self.stop:
            return None
        if self.trace:
            print("OP", self.nops, eng, getattr(fn, "desc", "?"), [getattr(x, "name", None) or x.b.name for x in writes], inc)
        reads = [x.b if isinstance(x, T) else x for x in reads]
        writes = [x.b if isinstance(x, T) else x for x in writes]
        waits = self._waits(eng, reads, writes, extra)
        tok = (self.sem[eng], self.cnt[eng] + 1)
        if inc:
            self.cnt[eng] += 1
        self.ops[eng].append((waits, fn, (self.sem[eng], 1) if inc else None))
        for b in reads:
            b.r.append(tok)
            if len(b.r) > 64:
                b.r = b.r[-64:] if False else _compact(b.r)
        for b in writes:
            b.w = tok
            b.r = []
        self.last[eng] = tok
        return tok

    def dma(self, q, out_ap, in_ap, sbuf, is_load, reads=(), writes=(), extra=(), **kw):
        self.nops += 1
        if self.stop is not None and self.nops > self.stop:
            return None
        sbuf = sbuf.b if isinstance(sbuf, T) else sbuf
        if sbuf.dsem is None:
            sbuf.dsem = self.nc.alloc_semaphore(name="dsem_%d" % self.nsem)
            self.nsem += 1
            self.dbufs.append(sbuf)
        rd = [x.b if isinstance(x, T) else x for x in reads]
        wr = [x.b if isinstance(x, T) else x for x in writes]
        if is_load:
            wr.append(sbuf)
        else:
            rd.append(sbuf)
        if q == "pool" and self.last_swdge is not None:
            extra = list(extra) + [self.last_swdge]
        waits = self._waits(q, rd, wr, extra)
        sbuf.dcnt += 1
        tok = (sbuf.dsem, 16 * sbuf.dcnt)
        self.ops[q].append((waits, I("dma_start", out_ap, in_ap, **kw), (sbuf.dsem, 16)))
        if q == "pool":
            self.last_swdge = tok
        for b in rd:
            b.r.append(tok)
            if len(b.r) > 64:
                b.r = _compact(b.r)
        for b in wr:
            b.w = tok
            b.r = []
        return tok

    def wait_all(self, eng, toks):
        waits = self._waits(eng, (), (), extra=toks)
        if waits:
            self.ops[eng].append((waits, None, None))

    def all_last(self):
        return [t for t in self.last.values()]

    def finish(self, eng="sp"):
        toks = self.all_last() + [(b.dsem, 16 * b.dcnt) for b in self.dbufs]
        self.wait_all(eng, toks)

    def emit(self):
        nc = self.nc
        with nc.Block() as block:
            def run(engname):
                def f(e):
                    for waits, fn, inc in self.ops[engname]:
                        for s, v in waits:
                            e.wait_ge(s, v)
                        if fn is None:
                            continue
                        ins = fn(e)
                        if inc is not None:
                            ins.then_inc(inc[0], inc[1])
                return f
            block.tensor(run("pe"))
            block.scalar(run("act"))
            block.vector(run("dve"))
            block.gpsimd(run("pool"))
            block.sync(run("sp"))


def _compact(toks):
    best = {}
    for s, v in toks:
        k = id(s)
        if k not in best or best[k][1] < v:
            best[k] = (s, v)
    return list(best.values())


def I(name, *a, **k):
    f = lambda e: getattr(e, name)(*a, **k)
    f.desc = name
    return f


class Ring:
    def __init__(self, items):
        self.items = items
        self.i = 0

    def next(self):
        x = self.items[self.i % len(self.items)]
        self.i += 1
        return x


def _t5_bucket_np(rel):
    nb = 16
    max_exact = 8
    side = np.where(rel > 0, nb, 0)
    n = np.abs(rel)
    nf = np.maximum(n, 1).astype(np.float32)
    large = max_exact + (np.log(nf / np.float32(max_exact)) / np.float32(math.log(128 / max_exact))
                         * np.float32(nb - max_exact)).astype(np.int32)
    large = np.minimum(large, nb - 1)
    return side + np.where(n < max_exact, n, large)


def _attn_consts():
    kp = np.arange(128)[:, None]
    ql = np.arange(512)[None, :]
    cm = np.zeros((128, 17, 512), np.float32)
    for r in range(4):
        kl = 128 * r + kp
        vis = (kl // 64) <= (ql // 64)
        cm[:, r, :] = vis
        cm[:, 4 + r, :] = kl < ql
        cm[:, 8 + r, :] = np.where(vis, 0.0, NEG_BIG)
    buckets = []
    for i, r in enumerate(range(-1, 4)):
        rel = 128 * r + kp - ql
        bi = _t5_bucket_np(rel)
        cm[:, 12 + i, :] = bi
        if r >= 0:
            vis = ((128 * r + kp) // 64) <= (ql // 64)
            present = sorted(set(bi[vis].tolist()))
        else:
            present = sorted(set(bi.reshape(-1).tolist()))
        buckets.append(present)
    mats = np.zeros((128, 3, 128), np.float32)
    mats[:, 0, :] = 1.0
    j = np.arange(128)[:, None]
    k = np.arange(128)[None, :]
    mats[:, 1, :] = np.where(j >= k, -1.0, 0.0)
    mats[:, 2, :] = -1.0
    ropec = np.zeros((96, 2), np.float32)
    inv = (10000.0 ** (-np.arange(16, dtype=np.float32) / 16)).astype(np.float32)
    ropec[64:96, 0] = np.concatenate([inv, inv])
    ropec[64:80, 1] = -1.0
    ropec[80:96, 1] = 1.0
    return cm, buckets, mats, ropec


_CM, _BUCKETS, _MATS, _ROPEC = _attn_consts()
_BIDX = _CM[:, 12:17, :].astype(np.int64)

W_CQ0, W_CQ1, W_CKV, W_KR, W_KRS, W_QD, W_KD, W_VD, W_QS, W_KS, W_VS, W_END = (
    0, 128, 192, 320, 416, 512, 640, 768, 896, 1024, 1152, 1216)


def build_A():
    nc = bass.Bass("TRN2", target_bir_lowering=False)
    S = SEQ
    import os
    NCH = int(os.environ.get('K_NCH', S // 512))
    PHASES = os.environ.get('K_PH', 'mds')
    DBG = os.environ.get('K_DBG', '')

    def din(name, shape, dt=F32):
        return nc.dram_tensor(name, list(shape), dt, kind="ExternalInput").ap()

    hT = din("hT", [1024, S])
    wall = din("wall", [1024, W_END])
    wuq = din("wuq", [192, 192])
    wukv = din("wukv", [128, 128])
    gq = din("gq", [192, 1])
    gkv = din("gkv", [128, 1])
    pos = din("pos", [1, S], I32)
    ropec = din("ropec", [96, 2])
    lamv = din("lamv", [1, 256])
    lamc = din("lamc", [128, 2])
    subln = din("subln", [128, 1])
    rbj = din("rbj", [1, 32])
    cmask = din("cmask", [128, 17, 512])
    biasg = din("biasg", [128, 5, 512])
    cmats = din("cmats", [128, 3, 128])
    yT = nc.dram_tensor("yT", [256, S], BF16, kind="ExternalOutput").ap()

    P = Prog(nc)
    with ExitStack() as es:
        def sb(name, shape, dt=F32):
            return T(es.enter_context(nc.sbuf_tensor(name, list(shape), dt)), name)

        def ps(name):
            return T(es.enter_context(nc.psum_tensor(name, [128, 512], F32)), name)

        rot = Ring([ps("rot%d" % i) for i in range(4)])
        acc = [ps("acc%d" % i) for i in range(4)]

        wallb = sb("wallb", [128, 8, W_END], BF16)
        wuq0f = sb("wuq0f", [128, 192]); wuq1f = sb("wuq1f", [64, 192])
        wuq0 = sb("wuq0", [128, 192], BF16); wuq1 = sb("wuq1", [64, 192], BF16)
        wukvb = sb("wukvb", [128, 128], BF16)
        gq0 = sb("gq0", [128, 1]); gq1 = sb("gq1", [64, 1]); gkvt = sb("gkvt", [128, 1])
        ropect = sb("ropect", [96, 2])
        lamt = sb("lamt", [128, 256]); lamct = sb("lamct", [128, 2]); sublnt = sb("sublnt", [128, 1])
        lams = sb("lams", [128, 8])
        rbt = sb("rbt", [128, 32])
        matsb = sb("matsb", [128, 3, 128], BF16)
        maskb = sb("maskb", [128, 8, 512], BF16)
        biasm = sb("biasm", [128, 5, 512])
        KTm = sb("KTm", [96, S], BF16); KTd = sb("KTd", [128, S], BF16); KTs = sb("KTs", [128, S // 2], BF16)
        Vm = sb("Vm", [128, S // 128, 65], BF16); Vd = sb("Vd", [128, S // 128, 128], BF16)
        onesf = sb("onesf", [128, 128])
        dacc = [sb("dacc%d" % i, [128, 512]) for i in range(2)]
        P.op("pool", I("memset", onesf[:], 1.0), [], [onesf])
        P.op("pool", I("memset", Vm[:, :, 64:65], 1.0), [], [Vm])
        Vs = sb("Vs", [128, S // 128, 64], BF16)
        kvb = [Buf("kv%d" % i) for i in range(NCH)]

        ones_b = matsb[:, 0, :]
        negTp = matsb[:, 1, :]
        negones = matsb[:, 2, :]

        if 'w' not in DBG:
            P.dma("pool", wallb[:], wall.rearrange("(kc p) n -> p kc n", p=128), wallb, True)
        P.dma("sp", wuq0f[:], wuq[0:128, :], wuq0f, True)
        P.dma("sp", wuq1f[:], wuq[128:192, :], wuq1f, True)
        P.dma("pool", wukvb[:], wukv[:, :], wukvb, True)
        P.dma("sp", gq0[:], gq[0:128, :], gq0, True)
        P.dma("sp", gq1[:], gq[128:192, :], gq1, True)
        P.dma("sp", gkvt[:], gkv[:, :], gkvt, True)
        P.dma("sp", ropect[:], ropec[:, :], ropect, True)
        P.dma("sp", lamt[:], lamv[0:1, :].partition_broadcast(128), lamt, True)
        P.dma("sp", lamct[:], lamc[:, :], lamct, True)
        P.dma("sp", sublnt[:], subln[:, :], sublnt, True)
        P.dma("sp", rbt[:], rbj[0:1, :].partition_broadcast(128), rbt, True)
        P.dma("pool", matsb[:], cmats[:, :, :], matsb, True)
        if 'k' not in DBG:
            P.dma("pool", maskb[:], cmask[:, 0:8, :], maskb, True)

        P.op("dve", I("tensor_scalar", wuq0[:], wuq0f[:], gq0[:, 0:1], None, ALU.mult), [wuq0f, gq0], [wuq0])
        P.op("dve", I("tensor_scalar", wuq1[:], wuq1f[:], gq1[:, 0:1], None, ALU.mult), [wuq1f, gq1], [wuq1])

        ltmp = sb("ltmp", [128, 128])
        P.op("dve", I("tensor_tensor", ltmp[:, 0:64], lamt[:, 0:64], lamt[:, 64:128], ALU.mult), [lamt], [ltmp])
        P.op("dve", I("tensor_tensor", ltmp[:, 64:128], lamt[:, 128:192], lamt[:, 192:256], ALU.mult), [lamt, ltmp], [ltmp])
        P.op("dve", I("tensor_reduce", lams[:, 2:4], ltmp[:].rearrange("p (a b) -> p a b", b=64), AX.X, ALU.add), [ltmp], [lams])
        P.op("act", I("activation", lams[:, 4:6], lams[:, 2:4], AF.Exp), [lams], [lams])
        P.op("dve", I("tensor_tensor", lams[:, 6:7], lams[:, 5:6], lams[:, 4:5], ALU.subtract), [lams], [lams])
        P.op("dve", I("tensor_tensor", lams[:, 0:1], lams[:, 6:7], lamct[:, 0:1], ALU.subtract), [lams, lamct], [lams])
        P.op("dve", I("tensor_tensor", lams[:, 1:2], sublnt[:, 0:1], lamct[:, 1:2], ALU.mult), [sublnt, lamct, lams], [lams])

        f1 = sb("f1", [128, 512]); f2 = sb("f2", [128, 512]); f3 = sb("f3", [128, 512]); f4 = sb("f4", [128, 512])
        bit = Ring([f1, f2])
        if 'b' not in DBG:
            P.dma("sp", biasm[:], biasg[:, :, :], biasm, True)
            for i in range(1, 5):
                bi = bit.next()
                P.dma("sp", bi[:], cmask[:, 8 + (i - 1), :], bi, True)
                P.op("dve", I("tensor_tensor", biasm[:, i, :], biasm[:, i, :], bi[:], ALU.add), [bi, biasm], [biasm])

        hTb = Ring([sb("hTb%d" % i, [128, 8, 512], BF16) for i in range(2)])
        posi = sb("posi", [96, 512], I32)
        ra = f1; rb_ = f2; rc = f3
        ri = posi
        Ct = Ring([sb("Ct%d" % i, [96, 512]) for i in range(1)])
        St = Ring([sb("St%d" % i, [96, 512]) for i in range(1)])
        cqb0 = sb("cqb0", [128, 512], BF16); cqb1 = sb("cqb1", [64, 512], BF16)
        sq0 = sb("sq0", [128, 512], BF16); sq1 = sb("sq1", [64, 512], BF16); sqkv = sb("sqkv", [128, 512], BF16)
        ckvf = sb("ckvf", [128, 512]); rstdq = sb("rstdq", [128, 512]); rstdkv = sb("rstdkv", [128, 512])
        ckvn = sb("ckvn", [128, 512], BF16)
        t1 = sb("t1", [96, 512]); t2 = sb("t2", [96, 512])
        QTm = Ring([sb("QTm%d" % i, [96, 512], BF16) for i in range(2)])
        QTd = Ring([sb("QTd%d" % i, [128, 2, 512], BF16) for i in range(2)])
        for t_ in QTd.items:
            P.op("pool", I("memset", t_[:], 0.0), [], [t_])
        QTs = Ring([sb("QTs%d" % i, [128, 2, 512], BF16) for i in range(2)])
        for t_ in QTs.items:
            P.op("pool", I("memset", t_[:], 0.0), [], [t_])
        aTr = Ring([sb("aT%d" % i, [128, 512], BF16) for i in range(4)])
        aTm = Ring([sb("aTm%d" % i, [128, 512], BF16) for i in range(3)])
        tmpr = Ring([sb("tmpb%d" % i, [128, 512]) for i in range(2)])
        er = Ring([sb("e%d" % i, [128, 512]) for i in range(2)])
        spr = Ring([sb("sp%d" % i, [128, 512], BF16) for i in range(4)])
        wr = Ring([sb("w%d" % i, [128, 512], BF16) for i in range(3)])
        srf = sb("srf", [128, 512])
        srb = Ring([sb("srb%d" % i, [128, 512], BF16) for i in range(4)])
        fsq = sb("fsq", [128, 512], BF16)
        yom = Ring([sb("yo%d" % i, [128, 512], BF16) for i in range(3)])
        yod = yom
        yos = yom

        hT_v = hT.rearrange("(kc p) t -> p kc t", p=128)

        def load_h(tc):
            hb = hTb.next()
            P.dma("pool", hb[:], hT_v[:, :, tc * 512:(tc + 1) * 512], hb, True)
            return hb

        def proj(hb, c0, c1, out_ps, M):
            for kc in range(8):
                P.op("pe", I("matmul", out_ps[0:M, :], wallb[:, kc, c0:c1], hb[:, kc, :], start=(kc == 0), stop=(kc == 7)),
                     [wallb, hb], [out_ps], inc=(kc == 7))

        def projv(hb, c0, c1, out_ps, N):
            for sub in range(4):
                for kc in range(8):
                    P.op("pe", I("matmul", out_ps[:, sub * N:(sub + 1) * N], hb[:, kc, sub * 128:(sub + 1) * 128],
                                                                   wallb[:, kc, c0:c1], start=(kc == 0), stop=(kc == 7)),
                         [wallb, hb], [out_ps], inc=(kc == 7 and sub == 3))

        def rstd_from(ssq_ps, out, dim):
            P.op("act", I("activation", out[:], ssq_ps[:], AF.Ln, scale=1.0 / dim, bias=EPS), [ssq_ps], [out])
            P.op("act", I("activation", out[:], out[:], AF.Exp, scale=-0.5), [out], [out])

        def rope_tables(tc):
            C = Ct.next(); Sg = St.next()
            R = slice(64, 96)
            P.dma("sp", posi[R, :], pos[0:1, tc * 512:(tc + 1) * 512].partition_broadcast(32), posi, True)
            P.op("dve", I("tensor_copy", ra[R, :], posi[R, :]), [posi], [ra])
            P.op("dve", I("tensor_scalar", ra[R, :], ra[R, :], ropect[R, 0:1], None, ALU.mult), [ra, ropect], [ra])

            def reduce_sin(dst, shift):
                P.op("dve", I("tensor_scalar", rb_[R, :], ra[R, :], shift, 1.0 / TWO_PI, ALU.add, ALU.mult), [ra], [rb_])
                P.op("dve", I("tensor_copy", ri[R, :], rb_[R, :]), [rb_], [ri])
                P.op("dve", I("tensor_copy", rb_[R, :], ri[R, :]), [ri], [rb_])
                P.op("dve", I("scalar_tensor_tensor", rc[R, :], rb_[R, :], -TWO_PI, ra[R, :], ALU.mult, ALU.add), [rb_, ra], [rc])
                P.op("dve", I("tensor_scalar", rc[R, :], rc[R, :], shift, None, ALU.add), [rc], [rc])
                P.op("dve", I("tensor_scalar", rb_[R, :], rc[R, :], float(np.pi), -TWO_PI, ALU.is_gt, ALU.mult), [rc], [rb_])
                P.op("dve", I("tensor_tensor", rc[R, :], rc[R, :], rb_[R, :], ALU.add), [rc, rb_], [rc])
                P.op("dve", I("tensor_scalar", rb_[R, :], rc[R, :], -float(np.pi), TWO_PI, ALU.is_lt, ALU.mult), [rc], [rb_])
                P.op("dve", I("tensor_tensor", rc[R, :], rc[R, :], rb_[R, :], ALU.add), [rc, rb_], [rc])
                P.op("act", I("activation", dst[R, :], rc[R, :], AF.Sin), [rc], [dst])

            reduce_sin(Sg, 0.0)
            P.op("dve", I("tensor_scalar", Sg[R, :], Sg[R, :], ropect[R, 1:2], None, ALU.mult), [Sg, ropect], [Sg])
            reduce_sin(C, float(np.pi / 2))
            return C, Sg

        def out_dma(src, rows, r0, tc):
            return P.dma("sp", yT[r0:r0 + rows, tc * 512:(tc + 1) * 512], src[0:rows, :], src, False)

        out_toks = []
        hb_next = load_h(0) if 'h' not in DBG else None
        for tc in range(NCH):
            hb = hb_next
            if tc + 1 < NCH:
                hb_next = load_h(tc + 1)
            t0 = tc * 512
            cs = slice(t0, t0 + 512)
            kvbuf = kvb[tc]
            C, Sg = rope_tables(tc)
            R = slice(64, 96)
            qm = QTm.next(); qd = QTd.next(); qs = QTs.next()

            p_cq0 = rot.next(); proj(hb, W_CQ0, W_CQ0 + 128, p_cq0, 128)
            P.op("dve", I("tensor_copy", cqb0[:], p_cq0[:]), [p_cq0], [cqb0])
            P.op("act", I("activation", sq0[:], cqb0[:], AF.Square), [cqb0], [sq0])
            p_cq1 = rot.next(); proj(hb, W_CQ1, W_CQ1 + 64, p_cq1, 64)
            P.op("dve", I("tensor_copy", cqb1[:], p_cq1[0:64, :]), [p_cq1], [cqb1])
            P.op("act", I("activation", sq1[:], cqb1[:], AF.Square), [cqb1], [sq1])
            p_ckv = rot.next(); proj(hb, W_CKV, W_CKV + 128, p_ckv, 128)
            P.op("dve", I("tensor_copy", ckvf[:], p_ckv[:]), [p_ckv], [ckvf])
            P.op("act", I("activation", sqkv[:], ckvf[:], AF.Square), [ckvf], [sqkv])
            p_ssq = rot.next()
            P.op("pe", I("matmul", p_ssq[:], ones_b, sq0[:], start=True, stop=False), [matsb, sq0], [p_ssq], inc=False)
            P.op("pe", I("matmul", p_ssq[:], matsb[0:64, 0, :], sq1[:], start=False, stop=True), [matsb, sq1], [p_ssq])
            rstd_from(p_ssq, rstdq, 192.0)
            p_ssk = rot.next()
            P.op("pe", I("matmul", p_ssk[:], ones_b, sqkv[:], start=True, stop=True), [matsb, sqkv], [p_ssk])
            rstd_from(p_ssk, rstdkv, 128.0)
            P.op("dve", I("scalar_tensor_tensor", ckvn[:], ckvf[:], gkvt[:, 0:1], rstdkv[:], ALU.mult, ALU.mult), [ckvf, gkvt, rstdkv], [ckvn])
            p_kr = rot.next(); proj(hb, W_KR, W_KR + 96, p_kr, 96)
            P.op("dve", I("tensor_tensor", t1[R, :], p_kr[R, :], C[R, :], ALU.mult), [p_kr, C], [t1])
            p_krs = rot.next(); proj(hb, W_KRS, W_KRS + 96, p_krs, 96)
            P.op("dve", I("tensor_tensor", t2[R, :], p_krs[R, :], Sg[R, :], ALU.mult), [p_krs, Sg], [t2])
            P.op("dve", I("tensor_tensor", KTm[R, cs], t1[R, :], t2[R, :], ALU.add), [t1, t2], [kvbuf])
            sc_m = 96.0 ** -0.5
            p_q = rot.next()
            P.op("pe", I("matmul", p_q[0:96, :], wuq0[:, 0:96], cqb0[:], start=True, stop=False), [wuq0, cqb0], [p_q], inc=False)
            P.op("pe", I("matmul", p_q[0:96, :], wuq1[:, 0:96], cqb1[:], start=False, stop=True), [wuq1, cqb1], [p_q])
            P.op("dve", I("scalar_tensor_tensor", qm[0:64, :], p_q[0:64, :], sc_m, rstdq[0:64, :], ALU.mult, ALU.mult), [p_q, rstdq], [qm])
            P.op("dve", I("tensor_tensor", t1[R, :], p_q[R, :], C[R, :], ALU.mult), [p_q, C], [t1])
            p_qs = rot.next()
            P.op("pe", I("matmul", p_qs[0:96, :], wuq0[:, 96:192], cqb0[:], start=True, stop=False), [wuq0, cqb0], [p_qs], inc=False)
            P.op("pe", I("matmul", p_qs[0:96, :], wuq1[:, 96:192], cqb1[:], start=False, stop=True), [wuq1, cqb1], [p_qs])
            P.op("dve", I("tensor_tensor", t2[R, :], p_qs[R, :], Sg[R, :], ALU.mult), [p_qs, Sg], [t2])
            P.op("dve", I("tensor_tensor", t1[R, :], t1[R, :], t2[R, :], ALU.add), [t1, t2], [t1])
            P.op("dve", I("scalar_tensor_tensor", qm[R, :], t1[R, :], sc_m, rstdq[R, :], ALU.mult, ALU.mult), [t1, rstdq], [qm])
            p_kn = rot.next()
            P.op("pe", I("matmul", p_kn[0:64, :], wukvb[:, 0:64], ckvn[:], start=True, stop=True), [wukvb, ckvn], [p_kn])
            P.op("dve", I("tensor_copy", KTm[0:64, cs], p_kn[0:64, :]), [p_kn], [kvbuf])
            p_vm = rot.next()
            for sub in range(4):
                P.op("pe", I("matmul", p_vm[:, sub * 64:(sub + 1) * 64], ckvn[:, sub * 128:(sub + 1) * 128], wukvb[:, 64:128], start=True, stop=True),
                     [wukvb, ckvn], [p_vm], inc=(sub == 3))
            P.op("dve", I("tensor_copy", Vm[:, tc * 4:(tc + 1) * 4, 0:64], p_vm[:, 0:256].rearrange("p (a b) -> p a b", b=64)), [p_vm, Vm], [kvbuf])
            p_qd = rot.next(); proj(hb, W_QD, W_QD + 128, p_qd, 128)
            P.op("dve", I("tensor_scalar", qd[0:64, 0, :], p_qd[0:64, :], 0.125, None, ALU.mult), [p_qd], [qd])
            P.op("dve", I("tensor_scalar", qd[64:128, 1, :], p_qd[64:128, :], 0.125, None, ALU.mult), [p_qd], [qd])
            p_kd = rot.next(); proj(hb, W_KD, W_KD + 128, p_kd, 128)
            P.op("dve", I("tensor_copy", KTd[:, cs], p_kd[:]), [p_kd], [kvbuf])
            p_vd = rot.next(); projv(hb, W_VD, W_VD + 128, p_vd, 128)
            P.op("dve", I("tensor_copy", Vd[:, tc * 4:(tc + 1) * 4, :], p_vd[:].rearrange("p (a b) -> p a b", b=128)), [p_vd], [kvbuf])
            p_qs2 = rot.next(); proj(hb, W_QS, W_QS + 128, p_qs2, 128)
            P.op("dve", I("tensor_scalar", qs[0:64, 0, :], p_qs2[0:64, :], 0.125, None, ALU.mult), [p_qs2], [qs])
            P.op("dve", I("tensor_scalar", qs[64:128, 1, :], p_qs2[64:128, :], 0.125, None, ALU.mult), [p_qs2], [qs])
            p_ks = rot.next(); proj(hb, W_KS, W_KS + 128, p_ks, 128)
            for jj in range(2):
                pc = (2 * tc + jj) * 128
                P.op("dve", I("tensor_copy", KTs[0:64, pc:pc + 128], p_ks[0:64, 256 * jj:256 * jj + 128]), [p_ks], [kvbuf])
                P.op("dve", I("tensor_copy", KTs[64:128, pc:pc + 128], p_ks[64:128, 256 * jj + 128:256 * jj + 256]), [p_ks], [kvbuf])
            p_vs = rot.next(); projv(hb, W_VS, W_VS + 64, p_vs, 64)
            P.op("dve", I("tensor_copy", Vs[:, tc * 4:(tc + 1) * 4, :], p_vs[:, 0:256].rearrange("p (a b) -> p a b", b=64)), [p_vs], [kvbuf])

            nkb = 4 * tc + 4
            LOOK = 2

            def pipeline(items, front, back, per_step=1):
                pend = []
                for n_, it in enumerate(items):
                    pend.append((it, front(it)))
                    if len(pend) > LOOK + per_step - 1:
                        back(*pend.pop(0))
                    if n_ % per_step == per_step - 1:
                        yield
                while pend:
                    back(*pend.pop(0))
                    yield

            def mla_gen(tc=tc, nkb=nkb, qm=qm):
                aO = acc[0]

                def m_front(kb):
                    r = kb - 4 * tc
                    ks = slice(kb * 128, (kb + 1) * 128)
                    sc = rot.next()
                    P.op("pe", I("matmul", sc[:], KTm[0:96, ks], qm[0:96, :], start=True, stop=True), [kvb[kb // 4], qm], [sc])
                    aT = aTm.next()
                    P.op("act", I("activation", aT[:], sc[:], AF.Exp), [sc], [aT])
                    if r >= 0:
                        P.op("pool", I("tensor_tensor", aT[:], aT[:], maskb[:, r, :], ALU.mult), [aT, maskb], [aT])
                    return aT

                def m_back(kb, aT):
                    P.op("pe", I("matmul", aO[0:65, :], Vm[:, kb, :], aT[:], start=(kb == 0), stop=(kb == nkb - 1)),
                         [kvb[kb // 4], aT, Vm], [aO])

                yield from pipeline(range(nkb), m_front, m_back)
                yo = yom.next()
                P.op("dve", I("reciprocal", f1[64:65, :], aO[64:65, :]), [aO], [f1])
                bc = rot.next()
                P.op("pe", I("matmul", bc[0:64, :], onesf[64:65, 0:64], f1[64:65, :], start=True, stop=True), [onesf, f1], [bc])
                P.op("act", I("copy", f2[0:64, :], bc[0:64, :]), [bc], [f2])
                P.op("dve", I("tensor_tensor", yo[0:64, :], aO[0:64, :], f2[0:64, :], ALU.mult), [aO, f2], [yo])
                out_toks.append(out_dma(yo, 64, 0, tc))

            def diff_gen(tc=tc, nkb=nkb, qd=qd):
                def d_front(it):
                    kb, i = it
                    r = kb - 4 * tc
                    ks = slice(kb * 128, (kb + 1) * 128)
                    PR = slice(64 * i, 64 * i + 64)
                    sc = rot.next()
                    P.op("pe", I("matmul", sc[:], KTd[:, ks], qd[:, i, :], start=True, stop=True), [kvb[kb // 4], qd], [sc])
                    aT = aTr.next()
                    if r <= -2:
                        P.op("act", I("activation", aT[:], sc[:], AF.Exp, bias=rbt[:, 15:16]), [sc, rbt], [aT])
                    else:
                        tb = tmpr.next()
                        P.op("dve", I("tensor_tensor", tb[:], sc[:], biasm[:, r + 1, :], ALU.add), [sc, biasm], [tb])
                        P.op("act", I("activation", aT[:], tb[:], AF.Exp), [tb], [aT])
                    return aT

                def d_back(it, aT):
                    kb, i = it
                    P.op("pe", I("matmul", acc[1 + i][:], Vd[:, kb, :], aT[:], start=(kb == 0), stop=(kb == nkb - 1)),
                         [kvb[kb // 4], aT], [acc[1 + i]])
                    eng = "dve" if i == 0 else "pool"
                    if kb == 0:
                        P.op(eng, I("tensor_copy", dacc[i][:], aT[:]), [aT], [dacc[i]])
                    else:
                        P.op(eng, I("tensor_tensor", dacc[i][:], dacc[i][:], aT[:], ALU.add), [aT, dacc[i]], [dacc[i]])

                yield from pipeline([(kb, i) for kb in range(nkb) for i in range(2)], d_front, d_back, per_step=2)
                den = [rot.next(), rot.next()]
                for i in range(2):
                    P.op("pe", I("matmul", den[i][:], onesf[:], dacc[i][:], start=True, stop=True), [onesf, dacc[i]], [den[i]])
                P.op("dve", I("reciprocal", f1[:], den[0][:]), [den[0]], [f1])
                P.op("dve", I("tensor_tensor", f2[:], acc[1][:], f1[:], ALU.mult), [acc[1], f1], [f2])
                P.op("dve", I("reciprocal", f3[:], den[1][:]), [den[1]], [f3])
                P.op("dve", I("tensor_tensor", f4[:], acc[2][:], f3[:], ALU.mult), [acc[2], f3], [f4])
                P.op("dve", I("scalar_tensor_tensor", f2[:], f4[:], lams[:, 0:1], f2[:], ALU.mult, ALU.add), [f4, lams, f2], [f2])
                P.op("act", I("activation", fsq[:], f2[:], AF.Square), [f2], [fsq])
                p_s = rot.next()
                P.op("pe", I("matmul", p_s[:], ones_b, fsq[:], start=True, stop=True), [matsb, fsq], [p_s])
                rstd_from(p_s, f1, 128.0)
                yo = yod.next()
                P.op("dve", I("scalar_tensor_tensor", yo[:], f2[:], lams[:, 1:2], f1[:], ALU.mult, ALU.mult), [f2, lams, f1], [yo])
                out_toks.append(out_dma(yo, 128, 64, tc))

            def sb_gen(tc=tc, nkb=nkb, qs=qs):
                aO = acc[3]
                state = {"srb": None}

                def s_front(kb):
                    r = kb - 4 * tc
                    ks = slice(kb * 128, (kb + 1) * 128)
                    z = rot.next()
                    kp = slice((kb // 2) * 128, (kb // 2) * 128 + 128)
                    P.op("pe", I("matmul", z[:], KTs[:, kp], qs[:, kb % 2, :], start=True, stop=True), [kvb[kb // 4], qs], [z])
                    ee = er.next()
                    P.op("act", I("activation", ee[:], z[:], AF.Exp), [z], [ee])
                    spt = spr.next()
                    P.op("act", I("activation", spt[:], ee[:], AF.Ln, bias=1.0), [ee], [spt])
                    if r >= 0:
                        P.op("pool", I("tensor_tensor", spt[:], spt[:], maskb[:, 4 + r, :], ALU.mult), [spt, maskb], [spt])
                    sb_in = state["srb"]
                    if kb > 0:
                        if kb == nkb - 1:
                            P.op("dve", I("tensor_copy", srf[:], spt[:]), [spt], [srf])
                        else:
                            P.op("dve", I("tensor_tensor", srf[:], srf[:], spt[:], ALU.add), [spt, srf], [srf])
                        nb = srb.next()
                        P.op("dve", I("tensor_copy", nb[:], srf[:]), [srf], [nb])
                        state["srb"] = nb
                    return spt, sb_in

                def s_back(kb, fr):
                    spt, sbc = fr
                    r = kb - 4 * tc
                    first = (kb == nkb - 1)
                    ks = slice(kb * 128, (kb + 1) * 128)
                    lw = rot.next()
                    kp = slice((kb // 2) * 128, (kb // 2) * 128 + 128)
                    P.op("pe", I("matmul", lw[:], KTs[:, kp], qs[:, kb % 2, :], start=True, stop=False), [kvb[kb // 4], qs], [lw], inc=False)
                    if first:
                        P.op("pe", I("matmul", lw[:], negTp, spt[:], start=False, stop=True), [matsb, spt], [lw])
                    else:
                        P.op("pe", I("matmul", lw[:], negTp, spt[:], start=False, stop=False), [matsb, spt], [lw], inc=False)
                        P.op("pe", I("matmul", lw[:], negones, sbc[:], start=False, stop=True), [matsb, sbc], [lw])
                    wt = wr.next()
                    P.op("act", I("activation", wt[:], lw[:], AF.Exp), [lw], [wt])
                    if r >= 0:
                        P.op("pool", I("tensor_tensor", wt[:], wt[:], maskb[:, 4 + r, :], ALU.mult), [wt, maskb], [wt])
                    return wt

                pend = []
                pend2 = []
                for kb in reversed(range(nkb)):
                    pend.append((kb, s_front(kb)))
                    if len(pend) > LOOK:
                        k2, fr = pend.pop(0)
                        pend2.append((k2, s_back(k2, fr)))
                    if len(pend2) > 1:
                        k3, wt = pend2.pop(0)
                        P.op("pe", I("matmul", aO[0:64, :], Vs[:, k3, :], wt[:], start=(k3 == nkb - 1), stop=(k3 == 0)),
                             [kvb[k3 // 4], wt], [aO])
                    yield
                while pend or pend2:
                    if pend:
                        k2, fr = pend.pop(0)
                        pend2.append((k2, s_back(k2, fr)))
                    if len(pend2) > 1 or not pend:
                        k3, wt = pend2.pop(0)
                        P.op("pe", I("matmul", aO[0:64, :], Vs[:, k3, :], wt[:], start=(k3 == nkb - 1), stop=(k3 == 0)),
                             [kvb[k3 // 4], wt], [aO])
                    yield
                yo = yos.next()
                P.op("dve", I("tensor_copy", yo[0:64, :], aO[0:64, :]), [aO], [yo])
                out_toks.append(out_dma(yo, 64, 192, tc))

            gens = []
            if 'm' in PHASES:
                gens.append(mla_gen())
            if 'd' in PHASES:
                gens.append(diff_gen())
            if 's' in PHASES:
                gens.append(sb_gen())
            while gens:
                for g_ in list(gens):
                    try:
                        next(g_)
                    except StopIteration:
                        gens.remove(g_)

        P.wait_all("sp", out_toks)
        P.finish()
        print("A nops", P.nops, {e: P.cnt[e] for e in P.cnt})
        P.emit()
    return nc


def build_B():
    nc = bass.Bass("TRN2", target_bir_lowering=False)
    NT = 2048
    NTT = NT // 128

    def din(name, shape, dt=F32):
        return nc.dram_tensor(name, list(shape), dt, kind="ExternalInput").ap()

    yT = din("yT", [1024, NT], BF16)
    h = din("h", [NT, 1024])
    pT = din("pT", [256, NT])
    w_o = din("w_o", [1024, 1024])
    ple_gate = din("ple_gate", [1024, 1024])
    ple_proj = din("ple_proj", [256, 1024])
    router_w = din("router_w", [1024, 16])
    router_b = din("router_b", [1, 16])
    lnp = din("lnp", [4, 1024])
    w_gate = din("w_gate", [16, 1024, 512])
    w_up = din("w_up", [16, 1024, 512])
    w_down = din("w_down", [16, 512, 1024])
    ident = din("ident", [128, 128])
    hout = nc.dram_tensor("hout", [NT, 1024], F32, kind="ExternalOutput").ap()

    P = Prog(nc)
    with ExitStack() as es:
        def sb(name, shape, dt=F32, stack=es):
            return T(stack.enter_context(nc.sbuf_tensor(name, list(shape), dt)), name)

        def ps(name):
            return T(es.enter_context(nc.psum_tensor(name, [128, 512], F32)), name)

        pss = [ps("ps%d" % i) for i in range(8)]

        R = sb("R", [128, NTT, 1024])
        Rb = [Buf("R%d" % i) for i in range(NTT)]
        h1T = sb("h1T", [128, 8, NT], BF16)
        h1Tb = [Buf("h1T%d" % i) for i in range(NTT)]
        G = sb("G", [128, NTT, 16])
        Gb = [Buf("G%d" % i) for i in range(NTT)]
        identt = sb("identt", [128, 128])
        lng = sb("lng", [128, 4, 1024])
        P.dma("sp", identt[:], ident[:, :], identt, True)
        for i in range(4):
            P.dma("sp", lng[:, i, :], lnp[i:i + 1, :].partition_broadcast(128), lng, True)

        def layer_norm(src, dst, gi, scr, stat):
            P.op("dve", I("tensor_reduce", stat[:, 0:1], src[:], AX.X, ALU.add), [src], [stat])
            P.op("dve", I("tensor_scalar", stat[:, 1:2], stat[:, 0:1], -1.0 / 1024, None, ALU.mult), [stat], [stat])
            P.op("dve", I("tensor_scalar", src[:], src[:], stat[:, 1:2], None, ALU.add), [src, stat], [src])
            P.op("act", I("activation", scr[:], src[:], AF.Square, accum_out=stat[:, 2:3]), [src, stat], [scr, stat])
            P.op("act", I("activation", stat[:, 3:4], stat[:, 2:3], AF.Ln, scale=1.0 / 1024, bias=EPS), [stat], [stat])
            P.op("act", I("activation", stat[:, 3:4], stat[:, 3:4], AF.Exp, scale=-0.5), [stat], [stat])
            P.op("dve", I("scalar_tensor_tensor", dst[:], src[:], stat[:, 3:4], lng[:, gi, :], ALU.mult, ALU.mult), [src, stat, lng], [dst])
            P.op("dve", I("tensor_tensor", dst[:], dst[:], lng[:, gi + 1, :], ALU.add), [dst, lng], [dst])

        with ExitStack() as s1:
            wob = sb("wob", [128, 8, 1024], BF16, s1)
            pgb = sb("pgb", [128, 8, 1024], BF16, s1)
            ppb = sb("ppb", [128, 2, 1024], BF16, s1)
            rwt = sb("rwt", [128, 8, 16], F32, s1)
            rbt = sb("rbt", [128, 16], F32, s1)
            P.dma("pool", wob[:], w_o.rearrange("(kc p) n -> p kc n", p=128), wob, True)
            P.dma("pool", pgb[:], ple_gate.rearrange("(kc p) n -> p kc n", p=128), pgb, True)
            P.dma("pool", ppb[:], ple_proj.rearrange("(kc p) n -> p kc n", p=128), ppb, True)
            P.dma("sp", rwt[:], router_w.rearrange("(kc p) n -> p kc n", p=128), rwt, True)
            P.dma("sp", rbt[:], router_b[0:1, :].partition_broadcast(128), rbt, True)
            yTb = Ring([sb("yTb%d" % i, [128, 8, 128], BF16, s1) for i in range(2)])
            pTb = Ring([sb("pTb%d" % i, [128, 2, 128], BF16, s1) for i in range(2)])
            pTf = Ring([sb("pTf%d" % i, [128, 2, 128], F32, s1) for i in range(2)])
            ht = Ring([sb("ht%d" % i, [128, 1024], F32, s1) for i in range(2)])
            pre = Ring([sb("pre%d" % i, [128, 1024], F32, s1) for i in range(2)])
            h1 = Ring([sb("h1_%d" % i, [128, 1024], F32, s1) for i in range(2)])
            scr = sb("scr", [128, 1024], F32, s1)
            h1Tf = Ring([sb("h1Tf%d" % i, [128, 8, 128], F32, s1) for i in range(2)])
            sg = sb("sg", [128, 1024], F32, s1)
            stat = Ring([sb("stat%d" % i, [128, 8], F32, s1) for i in range(2)])
            rt = [sb("rt%d" % i, [128, 16], F32, s1) for i in range(6)]
            rs = [sb("rs%d" % i, [128, 4], F32, s1) for i in range(6)]
            psr = Ring(pss)

            yT_v = yT.rearrange("(kc p) t -> p kc t", p=128)
            pT_v = pT.rearrange("(kc p) t -> p kc t", p=128)
            def b1_stage1(tt):
                ts_ = slice(tt * 128, (tt + 1) * 128)
                yb = yTb.next(); pb = pTb.next(); hh = ht.next()
                pf = pTf.next()
                P.dma("sp", yb[:], yT_v[:, :, ts_], yb, True)
                P.dma("sp", pf[:], pT_v[:, :, ts_], pf, True)
                P.op("pool", I("tensor_copy", pb[:], pf[:]), [pf], [pb])
                P.dma("sp", hh[:], h[ts_, :], hh, True)
                pr = pre.next(); st = stat.next(); hn = h1.next()
                for half in range(2):
                    pm = psr.next()
                    for kc in range(8):
                        P.op("pe", I("matmul", pm[:], yb[:, kc, :], wob[:, kc, half * 512:(half + 1) * 512],
                                                                                     start=(kc == 0), stop=(kc == 7)), [yb, wob], [pm], inc=(kc == 7))
                    P.op("dve", I("scalar_tensor_tensor",
                        pr[:, half * 512:(half + 1) * 512], hh[:, half * 512:(half + 1) * 512], ALPHA, pm[:], ALU.mult, ALU.add), [hh, pm], [pr])
                layer_norm(pr, hn, 0, scr, st)
                return pb, hn

            def b1_stage2(tt, pb, hn):
                ts_ = slice(tt * 128, (tt + 1) * 128)
                hf = h1Tf.next()
                for half in range(2):
                    pt = psr.next()
                    for k4 in range(4):
                        kc = half * 4 + k4
                        P.op("pe", I("transpose", pt[:, k4 * 128:(k4 + 1) * 128], hn[:, kc * 128:(kc + 1) * 128], identt[:]),
                             [hn, identt], [pt], inc=(k4 == 3))
                    P.op("act", I("copy", hf[:, half * 4:(half + 1) * 4, :], pt[:].rearrange("p (a b) -> p a b", b=128)), [pt], [hf])
                    P.op("dve", I("tensor_copy", h1T[:, half * 4:(half + 1) * 4, ts_], hf[:, half * 4:(half + 1) * 4, :]),
                         [hf], [h1Tb[tt]])
                pl = psr.next()
                for kc in range(8):
                    P.op("pe", I("matmul", pl[:, 0:16], hf[:, kc, :], rwt[:, kc, :], start=(kc == 0), stop=(kc == 7)),
                         [hf, rwt], [pl], inc=(kc == 7))
                sc_, sel, eq, g2, selm, wts = rt
                m1, m2, gs, ing, gmx, wsum = rs
                v3 = lambda a: a[:].rearrange("p (g k) -> p g k", k=4)
                b3 = lambda a: a[:, 0:4].unsqueeze(2).broadcast_to([128, 4, 4])
                P.op("act", I("activation", sc_[:], pl[:, 0:16], AF.Sigmoid), [pl], [sc_])
                P.op("dve", I("tensor_tensor", sel[:], sc_[:], rbt[:], ALU.add), [sc_, rbt], [sel])
                P.op("dve", I("tensor_reduce", m1[:], v3(sel), AX.X, ALU.max), [sel], [m1])
                P.op("dve", I("tensor_tensor", v3(eq), v3(sel), b3(m1), ALU.is_equal), [sel, m1], [eq])
                P.op("dve", I("scalar_tensor_tensor", g2[:], eq[:], -1e9, sel[:], ALU.mult, ALU.add), [eq, sel], [g2])
                P.op("dve", I("tensor_reduce", m2[:], v3(g2), AX.X, ALU.max), [g2], [m2])
                P.op("dve", I("tensor_tensor", gs[:], m1[:], m2[:], ALU.add), [m1, m2], [gs])
                P.op("dve", I("tensor_reduce", gmx[:, 0:1], gs[:], AX.X, ALU.max), [gs], [gmx])
                P.op("dve", I("tensor_scalar", ing[:], gs[:], gmx[:, 0:1], None, ALU.is_equal), [gs, gmx], [ing])
                P.op("dve", I("tensor_tensor", v3(selm), v3(sel), b3(m2), ALU.is_ge), [sel, m2], [selm])
                P.op("dve", I("tensor_tensor", v3(selm), v3(selm), b3(ing), ALU.mult), [selm, ing], [selm])
                P.op("dve", I("tensor_tensor", wts[:], sc_[:], selm[:], ALU.mult), [sc_, selm], [wts])
                P.op("dve", I("tensor_reduce", wsum[:, 0:1], wts[:], AX.X, ALU.add), [wts], [wsum])
                P.op("dve", I("reciprocal", wsum[:, 1:2], wsum[:, 0:1]), [wsum], [wsum])
                P.op("dve", I("tensor_scalar", G[:, tt, :], wts[:], wsum[:, 1:2], None, ALU.mult), [wts, wsum], [Gb[tt]])
                for half in range(2):
                    hs = slice(half * 512, (half + 1) * 512)
                    pg = psr.next()
                    for kc in range(8):
                        P.op("pe", I("matmul", pg[:], h1T[:, kc, ts_], pgb[:, kc, hs], start=(kc == 0), stop=(kc == 7)),
                             [h1Tb[tt], pgb], [pg], inc=(kc == 7))
                    pp = psr.next()
                    for kc in range(2):
                        P.op("pe", I("matmul", pp[:], pb[:, kc, :], ppb[:, kc, hs], start=(kc == 0), stop=(kc == 1)),
                             [pb, ppb], [pp], inc=(kc == 1))
                    P.op("act", I("activation", sg[:, hs], pg[:], AF.Sigmoid), [pg], [sg])
                    P.op("dve", I("tensor_tensor", sg[:, hs], sg[:, hs], pp[:], ALU.mult), [sg, pp], [sg])
                    P.op("dve", I("scalar_tensor_tensor", R[:, tt, hs], hn[:, hs], ALPHA, sg[:, hs], ALU.mult, ALU.add),
                         [hn, sg], [Rb[tt]])

            nxt1 = b1_stage1(0)
            for tt in range(NTT):
                cur1 = nxt1
                if tt + 1 < NTT:
                    nxt1 = b1_stage1(tt + 1)
                b1_stage2(tt, *cur1)
        barrier = P.all_last()

        with ExitStack() as s2:
            wgb = Ring([sb("wgb%d" % i, [128, 8, 512], BF16, s2) for i in range(2)])
            wub = Ring([sb("wub%d" % i, [128, 8, 512], BF16, s2) for i in range(2)])
            wdb = Ring([sb("wdb%d" % i, [128, 4, 1024], BF16, s2) for i in range(2)])
            hid = Ring([sb("hid%d" % i, [128, 4, 512], BF16, s2) for i in range(2)])
            sgr = Ring([sb("sgm%d" % i, [128, 512], F32, s2) for i in range(2)])
            psgu = Ring(pss[0:4])
            psd = Ring(pss[4:8])
            ob = Ring([sb("ob%d" % i, [128, 1024], F32, s2) for i in range(2)])
            scr2 = sb("scr2", [128, 1024], F32, s2)
            stat2 = Ring([sb("stat2_%d" % i, [128, 8], F32, s2) for i in range(2)])
            rtmp = Ring([sb("rtmp%d" % i, [128, 1024], F32, s2) for i in range(2)])
            toks = []
            b3_pend = []

            def b3_tile(tt):
                src = rtmp.next(); o = ob.next(); st = stat2.next()
                P.op("pool", I("tensor_copy", src[:], R[:, tt, :]), [Rb[tt]], [src])
                layer_norm(src, o, 2, scr2, st)
                toks.append(P.dma("sp", hout[tt * 128:(tt + 1) * 128, :], o[:], o, False))

            def load_w(e_):
                a, b, c = wgb.next(), wub.next(), wdb.next()
                P.dma("pool", a[:], w_gate[e_].rearrange("(kc p) n -> p kc n", p=128), a, True, extra=barrier)
                P.dma("pool", b[:], w_up[e_].rearrange("(kc p) n -> p kc n", p=128), b, True, extra=barrier)
                P.dma("pool", c[:], w_down[e_].rearrange("(kc p) n -> p kc n", p=128), c, True, extra=barrier)
                return a, b, c

            nxt = load_w(0)
            for e_ in range(16):
                wg_, wu_, wd_ = nxt
                if e_ + 1 < 16:
                    nxt = load_w(e_ + 1)
                for c4 in range(4):
                    cs = slice(c4 * 512, (c4 + 1) * 512)
                    hbufs = [h1Tb[c4 * 4 + i] for i in range(4)]
                    hd = hid.next()
                    for m in range(4):
                        ms = slice(m * 128, (m + 1) * 128)
                        pg = psgu.next(); pu = psgu.next()
                        for kc in range(8):
                            P.op("pe", I("matmul", pg[:], wg_[:, kc, ms], h1T[:, kc, cs], start=(kc == 0), stop=(kc == 7)),
                                 [wg_] + hbufs, [pg], inc=(kc == 7), extra=barrier if (e_ == 0 and c4 == 0 and m == 0 and kc == 0) else ())
                        for kc in range(8):
                            P.op("pe", I("matmul", pu[:], wu_[:, kc, ms], h1T[:, kc, cs], start=(kc == 0), stop=(kc == 7)),
                                 [wu_] + hbufs, [pu], inc=(kc == 7))
                        sg_ = sgr.next()
                        P.op("act", I("activation", sg_[:], pg[:], AF.Silu), [pg], [sg_])
                        P.op("dve", I("tensor_tensor", hd[:, m, :], sg_[:], pu[:], ALU.mult), [sg_, pu], [hd])
                        if b3_pend:
                            b3_tile(b3_pend.pop(0))
                    for t4 in range(4):
                        tt = c4 * 4 + t4
                        for half in range(2):
                            hs = slice(half * 512, (half + 1) * 512)
                            pd = psd.next()
                            for m in range(4):
                                P.op("pe", I("matmul", pd[:], hd[:, m, t4 * 128:(t4 + 1) * 128], wd_[:, m, hs],
                                                                                                        start=(m == 0), stop=(m == 3)), [hd, wd_], [pd], inc=(m == 3))
                            P.op("dve", I("scalar_tensor_tensor", R[:, tt, hs], pd[:], G[:, tt, e_:e_ + 1], R[:, tt, hs], ALU.mult, ALU.add),
                                 [pd, Gb[tt], Rb[tt]], [Rb[tt]])
                    if e_ == 15:
                        b3_pend.extend(c4 * 4 + t4 for t4 in range(4))
            while b3_pend:
                b3_tile(b3_pend.pop(0))
            P.wait_all("sp", toks)
        P.finish()
        P.emit()
    return nc


_PROGS = {}


def _prog(name):
    if name not in _PROGS:
        _PROGS[name] = build_A() if name == "A" else build_B()
    return _PROGS[name]


def _c(a):
    return np.ascontiguousarray(a, dtype=np.float32)


def _a_inputs(l, c, hT_b, inp):
    b, j = divmod(c, 4)
    w_in = inp["w_in"][l]
    MLA_IN = 352
    D0 = MLA_IN
    S0 = MLA_IN + 1536
    wall = np.zeros((1024, W_END), np.float32)
    wall[:, W_CQ0:W_CQ0 + 192] = w_in[:, 0:192]
    wall[:, W_CKV:W_CKV + 128] = w_in[:, 192:320]
    kr = w_in[:, 320:352]
    wall[:, W_KR + 64:W_KR + 96] = kr
    wall[:, W_KRS + 64:W_KRS + 96] = np.concatenate([kr[:, 16:32], kr[:, 0:16]], axis=1)
    wall[:, W_QD:W_QD + 128] = w_in[:, D0 + j * 128:D0 + (j + 1) * 128]
    wall[:, W_KD:W_KD + 128] = w_in[:, D0 + 512 + j * 128:D0 + 512 + (j + 1) * 128]
    wall[:, W_VD:W_VD + 128] = w_in[:, D0 + 1024 + j * 128:D0 + 1024 + (j + 1) * 128]
    for d_ in (0, 64):
        wall[:, W_QS + d_:W_QS + d_ + 64] = w_in[:, S0 + j * 64:S0 + (j + 1) * 64]
        wall[:, W_KS + d_:W_KS + d_ + 64] = w_in[:, S0 + 256 + j * 64:S0 + 256 + (j + 1) * 64]
    wall[:, W_VS:W_VS + 64] = w_in[:, S0 + 512 + j * 64:S0 + 512 + (j + 1) * 64]
    wq = inp["mla_w_uq"][l][:, j * 96:(j + 1) * 96]
    wuq = np.zeros((192, 192), np.float32)
    wuq[:, 0:96] = wq
    wuq[:, 96 + 64:96 + 80] = wq[:, 80:96]
    wuq[:, 96 + 80:96 + 96] = wq[:, 64:80]
    lam_init = 0.8 - 0.6 * math.exp(-0.3 * l)
    lamc = np.zeros((128, 2), np.float32)
    lamc[:, 0] = lam_init
    lamc[:, 1] = 1.0 - lam_init
    lamv = np.concatenate([inp["diff_lambda_q1"][l], inp["diff_lambda_k1"][l],
                           inp["diff_lambda_q2"][l], inp["diff_lambda_k2"][l]])[None, :]
    return {
        "hT": hT_b,
        "wall": wall,
        "wuq": wuq,
        "wukv": _c(inp["mla_w_ukv"][l][:, j * 128:(j + 1) * 128]),
        "gq": _c(inp["mla_q_norm"][l][:, None]),
        "gkv": _c(inp["mla_kv_norm"][l][:, None]),
        "pos": np.ascontiguousarray(inp["positions"][b][None, :], dtype=np.int32),
        "ropec": _ROPEC,
        "lamv": _c(lamv),
        "lamc": lamc,
        "subln": _c(inp["diff_subln"][l][:, None]),
        "rbj": _c(inp["rel_bias"][:, j][None, :]),
        "cmask": _CM,
        "biasg": _c(inp["rel_bias"][:, j][_BIDX]),
        "cmats": _MATS,
    }


def _b_inputs(l, c, YT, h_flat, inp):
    b, q = divmod(c, 4)
    ts_ = slice(q * 2048, (q + 1) * 2048)
    return {
        "yT": np.ascontiguousarray(YT[b][:, ts_]),
        "h": _c(h_flat[c * 2048:(c + 1) * 2048]),
        "pT": _c(inp["p"][l, b, ts_, :].T),
        "w_o": _c(inp["w_o"][l]),
        "ple_gate": _c(inp["ple_gate"][l]),
        "ple_proj": _c(inp["ple_proj"][l]),
        "router_w": _c(inp["router_w"]),
        "router_b": _c(inp["router_b"][None, :]),
        "lnp": _c(np.stack([inp["ln1_g"][l], inp["ln1_b"][l], inp["ln2_g"][l], inp["ln2_b"][l]])),
        "w_gate": _c(inp["w_gate"][l]),
        "w_up": _c(inp["w_up"][l]),
        "w_down": _c(inp["w_down"][l]),
        "ident": np.eye(128, dtype=np.float32),
    }


def run_A(l, h_flat, inp):
    hT = [_c(h_flat[b * SEQ:(b + 1) * SEQ].T) for b in range(BATCH)]
    in_maps = [_a_inputs(l, c, hT[c // 4], inp) for c in range(N_CORES)]
    res = run_bass_kernel_spmd(_prog("A"), in_maps, core_ids=list(range(N_CORES)))
    YT = [np.zeros((1024, SEQ), res.results[0]["yT"].dtype) for _ in range(BATCH)]
    for c in range(N_CORES):
        b, j = divmod(c, 4)
        y = res.results[c]["yT"]
        YT[b][j * 64:(j + 1) * 64] = y[0:64]
        YT[b][256 + j * 128:256 + (j + 1) * 128] = y[64:192]
        YT[b][768 + j * 64:768 + (j + 1) * 64] = y[192:256]
    return YT


def run_B(l, YT, h_flat, inp):
    in_maps = [_b_inputs(l, c, YT, h_flat, inp) for c in range(N_CORES)]
    res = run_bass_kernel_spmd(_prog("B"), in_maps, core_ids=list(range(N_CORES)))
    return np.concatenate([res.results[c]["hout"] for c in range(N_CORES)], axis=0)


def kernel(**inputs):
    inp = {k: np.asarray(v) for k, v in inputs.items()}
    h_flat = _c(inp["x"].reshape(BATCH * SEQ, D_MODEL))
    for l in range(DEPTH):
        YT = run_A(l, h_flat, inp)
        h_flat = run_B(l, YT, h_flat, inp)
    return h_flat.reshape(BATCH, SEQ, D_MODEL).astype(np.float32)
```
